# Optimizing a Trainium2 kernel written in Bass

```python
import math
import jax, jax.numpy as jnp
from jax import lax
import numpy as np

D_MODEL = 1024
BATCH = 16
SEQ = 4096
DEPTH = 1

HEAD_DIM = 64
N_Q_HEADS = 8
N_KV_HEADS = 2
Q_GROUP = N_Q_HEADS // N_KV_HEADS
ATTN_WIDTH = N_Q_HEADS * HEAD_DIM
KV_WIDTH = N_KV_HEADS * HEAD_DIM
WINDOW = 128
ATTN_BLOCK = 128
ROPE_DIM = HEAD_DIM // 4
ROPE_THETA = 500000.0
N_G_HEADS = 8
G_HEAD_DIM = 64
GMLP_WIDTH = N_G_HEADS * G_HEAD_DIM
CHUNK = 128
MIX_WIDTH = ATTN_WIDTH + GMLP_WIDTH
IN_WIDTH = ATTN_WIDTH + 2 * KV_WIDTH + 2 * GMLP_WIDTH
SPLITS = (ATTN_WIDTH, ATTN_WIDTH + KV_WIDTH, ATTN_WIDTH + 2 * KV_WIDTH,
          ATTN_WIDTH + 2 * KV_WIDTH + GMLP_WIDTH)
N_EXPERTS = 32
TOP_K = 4
D_EXPERT = D_MODEL
SWIGLU_LIMIT = 7.0
SWIGLU_ALPHA = 1.702
EXPERT_BLOCK = 256
LN_EPS = 1e-5
DEEPNORM_ALPHA = (2.0 * DEPTH) ** 0.25
DEEPNORM_BETA = (8.0 * DEPTH) ** -0.25

kernel_name = "hybrid_swa_sink_gmlp_moe_deepnorm"


def layer_norm(x, g, b):
    xf = x.astype(jnp.float32)
    mu = jnp.mean(xf, axis=-1, keepdims=True)
    var = jnp.mean(jnp.square(xf - mu), axis=-1, keepdims=True)
    y = (xf - mu) * lax.rsqrt(var + LN_EPS)
    return (y * g.astype(jnp.float32) + b.astype(jnp.float32)).astype(x.dtype)


def rotary_tables(positions):
    inv_freq = ROPE_THETA ** (-jnp.arange(0, ROPE_DIM, 2, dtype=jnp.float32) / ROPE_DIM)
    ang = positions.astype(jnp.float32)[..., None] * inv_freq
    return jnp.cos(ang)[:, :, None, :], jnp.sin(ang)[:, :, None, :]


def apply_partial_rope(t, cos, sin):
    half = ROPE_DIM // 2
    cos = cos.astype(t.dtype)
    sin = sin.astype(t.dtype)
    r1, r2, rest = t[..., :half], t[..., half:ROPE_DIM], t[..., ROPE_DIM:]
    return jnp.concatenate([r1 * cos - r2 * sin, r2 * cos + r1 * sin, rest], axis=-1)


def sliding_window_attention(q, k, v, sinks):
    B, S = q.shape[0], q.shape[1]
    nb = S // ATTN_BLOCK
    qb = q.reshape(B, nb, ATTN_BLOCK, N_KV_HEADS, Q_GROUP, HEAD_DIM)
    kb = k.reshape(B, nb, ATTN_BLOCK, N_KV_HEADS, HEAD_DIM)
    vb = v.reshape(B, nb, ATTN_BLOCK, N_KV_HEADS, HEAD_DIM)
    pad = ((0, 0), (1, 0), (0, 0), (0, 0), (0, 0))
    kw = jnp.concatenate([jnp.pad(kb[:, :-1], pad), kb], axis=2)
    vw = jnp.concatenate([jnp.pad(vb[:, :-1], pad), vb], axis=2)
    s = jnp.einsum('bnqhgd,bnkhd->bnhgqk', qb, kw).astype(jnp.float32) * (HEAD_DIM ** -0.5)
    qi = jnp.arange(ATTN_BLOCK)[:, None] + ATTN_BLOCK
    kj = jnp.arange(2 * ATTN_BLOCK)[None, :]
    diff = qi - kj
    band = (diff >= 0) & (diff < WINDOW)
    has_prev = (jnp.arange(nb)[:, None, None] > 0) | (kj[None] >= ATTN_BLOCK)
    mask = band[None] & has_prev
    s = jnp.where(mask[None, :, None, None], s, -jnp.inf)
    sink = sinks.astype(jnp.float32).reshape(N_KV_HEADS, Q_GROUP)[None, None, :, :, None, None]
    m = jnp.maximum(jnp.max(s, axis=-1, keepdims=True), sink)
    p = jnp.exp(s - m)
    p = p / (jnp.sum(p, axis=-1, keepdims=True) + jnp.exp(sink - m))
    o = jnp.einsum('bnhgqk,bnkhd->bnqhgd', p.astype(v.dtype), vw)
    return o.reshape(B, S, ATTN_WIDTH)


def chunked_spatial_gating(u, v, gn_g, gn_b, w_s, b_s):
    B, S = u.shape[0], u.shape[1]
    nc = S // CHUNK
    v = layer_norm(v, gn_g, gn_b)
    vc = v.reshape(B, nc, CHUNK, N_G_HEADS, G_HEAD_DIM)
    tril = jnp.tril(jnp.ones((CHUNK, CHUNK), dtype=bool))
    w = jnp.where(tril[None], w_s, jnp.zeros((), w_s.dtype))
    mixed = jnp.einsum('hts,bcshd->bcthd', w, vc) + b_s.T[None, None, :, :, None]
    return (u.reshape(B, nc, CHUNK, N_G_HEADS, G_HEAD_DIM) * mixed).reshape(B, S, GMLP_WIDTH)


def hybrid_mixer(x, cos, sin, w_in, b_in, sinks, gn_g, gn_b, w_s, b_s, w_out, b_out):
    B, S, _ = x.shape
    proj = jnp.einsum('bsd,de->bse', x, w_in) + b_in
    q, k, v, gu, gv = jnp.split(proj, SPLITS, axis=-1)
    q = apply_partial_rope(q.reshape(B, S, N_Q_HEADS, HEAD_DIM), cos, sin)
    k = apply_partial_rope(k.reshape(B, S, N_KV_HEADS, HEAD_DIM), cos, sin)
    v = v.reshape(B, S, N_KV_HEADS, HEAD_DIM)
    attn = sliding_window_attention(q, k, v, sinks)
    gu = jax.nn.gelu(gu, approximate=False).reshape(B, S, N_G_HEADS, G_HEAD_DIM)
    gv = jax.nn.gelu(gv, approximate=False).reshape(B, S, N_G_HEADS, G_HEAD_DIM)
    gmix = chunked_spatial_gating(gu, gv, gn_g, gn_b, w_s, b_s)
    o = jnp.concatenate([attn, gmix], axis=-1)
    return jnp.einsum('bse,ed->bsd', o, w_out) + b_out


def clamped_swiglu(gu):
    gate, up = gu[..., :D_EXPERT], gu[..., D_EXPERT:]
    gate = jnp.minimum(gate, SWIGLU_LIMIT)
    up = jnp.clip(up, -SWIGLU_LIMIT, SWIGLU_LIMIT)
    return (up + 1.0) * (gate * jax.nn.sigmoid(gate * SWIGLU_ALPHA))


def routed_experts(x, w_router, b_router, w_gate_up, b_gate_up, w_down, b_down):
    B, S, D = x.shape
    T = B * S
    x2 = x.reshape(T, D)
    logits = (x2 @ w_router + b_router).astype(jnp.float32)
    top_val, top_idx = lax.top_k(logits, TOP_K)
    gates = jax.nn.softmax(top_val, axis=-1).astype(x.dtype)
    N = T * TOP_K
    flat_e = top_idx.reshape(N)
    flat_tok = jnp.arange(N, dtype=jnp.int32) // TOP_K
    flat_g = gates.reshape(N)
    order = jnp.argsort(flat_e)
    sorted_e = flat_e[order]
    counts = jnp.bincount(flat_e, length=N_EXPERTS)
    starts = jnp.cumsum(counts) - counts
    padded = ((counts + EXPERT_BLOCK - 1) // EXPERT_BLOCK) * EXPERT_BLOCK
    pends = jnp.cumsum(padded)
    pstarts = pends - padded
    dest = pstarts[sorted_e] + (jnp.arange(N) - starts[sorted_e])
    NP = N + N_EXPERTS * EXPERT_BLOCK
    nblk = NP // EXPERT_BLOCK
    buf_tok = jnp.zeros((NP,), jnp.int32).at[dest].set(flat_tok[order])
    buf_g = jnp.zeros((NP,), x.dtype).at[dest].set(flat_g[order])
    blk_start = jnp.arange(nblk) * EXPERT_BLOCK
    blk_e = jnp.minimum(jnp.searchsorted(pends, blk_start, side='right'), N_EXPERTS - 1)

    def body(acc, blk):
        tok, g, e = blk
        xb = x2[tok]
        h = clamped_swiglu(xb @ w_gate_up[e] + b_gate_up[e])
        y = (h @ w_down[e] + b_down[e]) * g[:, None]
        return acc.at[tok].add(y.astype(acc.dtype)), None

    acc0 = jnp.zeros((T, D), x.dtype)
    out, _ = lax.scan(body, acc0, (buf_tok.reshape(nblk, EXPERT_BLOCK),
                                   buf_g.reshape(nblk, EXPERT_BLOCK), blk_e))
    return out.reshape(B, S, D)


def setup_inputs(seed: int = 0) -> dict:
    key = jax.random.key(seed)
    ks = jax.random.split(key, 24)
    f32 = jnp.float32
    L = DEPTH
    nrm = lambda k, shape, scale: jax.random.normal(k, shape, f32) * scale
    x = jax.random.normal(ks[0], (BATCH, SEQ, D_MODEL), f32)
    offsets = jax.random.randint(ks[1], (BATCH, 1), 0, 4096, dtype=jnp.int32)
    positions = offsets + jnp.arange(SEQ, dtype=jnp.int32)[None, :]
    col_scale = jnp.concatenate([jnp.ones((ATTN_WIDTH + KV_WIDTH,), f32),
                                 jnp.full((KV_WIDTH + 2 * GMLP_WIDTH,), DEEPNORM_BETA, f32)])
    return {
        "x": x,
        "positions": positions,
        "ln_in_g": 1.0 + nrm(ks[2], (D_MODEL,), 0.02),
        "ln_in_b": nrm(ks[3], (D_MODEL,), 0.02),
        "w_in": nrm(ks[4], (L, D_MODEL, IN_WIDTH), D_MODEL ** -0.5) * col_scale,
        "b_in": nrm(ks[5], (L, IN_WIDTH), 0.02),
        "sinks": nrm(ks[6], (L, N_Q_HEADS), 0.5),
        "gn_g": 1.0 + nrm(ks[7], (L, G_HEAD_DIM), 0.02),
        "gn_b": nrm(ks[8], (L, G_HEAD_DIM), 0.02),
        "w_s": nrm(ks[9], (L, N_G_HEADS, CHUNK, CHUNK), CHUNK ** -0.5),
        "b_s": 1.0 + nrm(ks[10], (L, N_G_HEADS, CHUNK), 0.02),
        "w_out": nrm(ks[11], (L, MIX_WIDTH, D_MODEL), MIX_WIDTH ** -0.5 * DEEPNORM_BETA),
        "b_out": nrm(ks[12], (L, D_MODEL), 0.02),
        "ln1_g": 1.0 + nrm(ks[13], (L, D_MODEL), 0.02),
        "ln1_b": nrm(ks[14], (L, D_MODEL), 0.02),
        "w_router": nrm(ks[15], (L, D_MODEL, N_EXPERTS), D_MODEL ** -0.5),
        "b_router": nrm(ks[16], (L, N_EXPERTS), 0.01),
        "w_gate_up": nrm(ks[17], (L, N_EXPERTS, D_MODEL, 2 * D_EXPERT), D_MODEL ** -0.5 * DEEPNORM_BETA),
        "b_gate_up": nrm(ks[18], (L, N_EXPERTS, 2 * D_EXPERT), 0.02),
        "w_down": nrm(ks[19], (L, N_EXPERTS, D_EXPERT, D_MODEL), D_EXPERT ** -0.5 * DEEPNORM_BETA),
        "b_down": nrm(ks[20], (L, N_EXPERTS, D_MODEL), 0.02),
        "ln2_g": 1.0 + nrm(ks[21], (L, D_MODEL), 0.02),
        "ln2_b": nrm(ks[22], (L, D_MODEL), 0.02),
    }


def reference(x, positions, ln_in_g, ln_in_b, w_in, b_in, sinks, gn_g, gn_b, w_s, b_s,
              w_out, b_out, ln1_g, ln1_b, w_router, b_router, w_gate_up, b_gate_up,
              w_down, b_down, ln2_g, ln2_b):
    cos, sin = rotary_tables(positions)
    h = layer_norm(x, ln_in_g, ln_in_b)
    for l in range(DEPTH):
        mix = hybrid_mixer(h, cos, sin, w_in[l], b_in[l], sinks[l], gn_g[l], gn_b[l],
                           w_s[l], b_s[l], w_out[l], b_out[l])
        h = layer_norm(DEEPNORM_ALPHA * h + mix, ln1_g[l], ln1_b[l])
        ffn = routed_experts(h, w_router[l], b_router[l], w_gate_up[l], b_gate_up[l],
                             w_down[l], b_down[l])
        h = layer_norm(DEEPNORM_ALPHA * h + ffn, ln2_g[l], ln2_b[l])
    return h
```

```python
import numpy as np
from contextlib import ExitStack
import concourse.bass as bass
import concourse.mybir as mybir
from concourse.bass_utils import run_bass_kernel_spmd

F32, BF16, I32 = mybir.dt.float32, mybir.dt.bfloat16, mybir.dt.int32
ALU = mybir.AluOpType
AF = mybir.ActivationFunctionType
AX = mybir.AxisListType

ENGS = ("pe", "dve", "act", "pool", "sp")
SEM_CHUNK = 12000
DMA_POOL = {"sp": 12, "pool": 2, "act": 8}


class Tk:
    __slots__ = ("eng", "dma", "needed", "sem", "val", "pos")


class Sched:
    def __init__(self):
        self.ops = {e: [] for e in ENGS}
        self.lastw = {}
        self.readers = {}

    def op(self, eng, fn, r=(), w=(), dma=False, extra=(), dyn=False):
        t = Tk()
        t.eng, t.dma, t.needed, t.sem, t.val = eng, dma, dma, None, None
        t.pos = len(self.ops[eng])
        waits = []

        def consider(x, writer):
            if x is None:
                return
            if not x.dma and x.eng == eng and (eng == "pe" or not writer):
                return
            waits.append(x)

        for k in r:
            consider(self.lastw.get(k), True)
        for k in w:
            consider(self.lastw.get(k), True)
            rd = self.readers.get(k)
            if rd:
                for y in rd[0].values():
                    consider(y, False)
                for y in rd[1]:
                    consider(y, False)
        for x in extra:
            consider(x, True)
        for x in waits:
            x.needed = True
        self.ops[eng].append((fn, waits, t, dyn))
        for k in r:
            rd = self.readers.setdefault(k, ({}, []))
            if dma:
                rd[1].append(t)
            else:
                rd[0][eng] = t
        for k in w:
            self.lastw[k] = t
            self.readers[k] = ({}, [])
        return t

    def finalize(self):
        self.final = {}
        for e in ENGS:
            n = 0
            nd = 0
            for (_fn, _waits, t, _dyn) in self.ops[e]:
                if t.dma:
                    R = DMA_POOL[e]
                    t.sem = ("d", e, nd % R)
                    t.val = 16 * (nd // R + 1)
                    nd += 1
                elif t.needed:
                    t.sem = ("c", e, n // SEM_CHUNK)
                    t.val = n % SEM_CHUNK + 1
                    n += 1
                if t.sem is not None:
                    self.final[t.sem] = max(self.final.get(t.sem, 0), t.val)
        return self.final

    def emit(self, eng, h, sems, g=None):
        waited = {}

        def do_wait(sem, val):
            if waited.get(sem, 0) >= val:
                return
            waited[sem] = val
            h.wait_ge(sems[sem], val)

        for (fn, waits, t, dyn) in self.ops[eng]:
            need = {}
            for x in waits:
                if need.get(x.sem, 0) < x.val:
                    need[x.sem] = x.val
            if t.dma and t.val > 16:
                if need.get(t.sem, 0) < t.val - 16:
                    need[t.sem] = t.val - 16
            for sem in sorted(need):
                do_wait(sem, need[sem])
            ins = fn(h, g) if dyn else fn(h)
            if t.sem is not None:
                ins.then_inc(sems[t.sem], 16 if t.dma else 1)

    def barrier(self, eng, h, sems, bar_a, bar_b, target):
        for sem in sorted(self.final):
            h.wait_ge(sems[sem], self.final[sem])
        h.nop().then_inc(bar_a, 1)
        h.wait_ge(bar_a, target)
        for sem in sorted(self.final):
            if sem[1] == eng:
                h.sem_clear(sems[sem])
        h.nop().then_inc(bar_b, 1)
        h.wait_ge(bar_b, target)


P = 128
D = 1024
KC = 8
SEQ = 4096
TOK = 8192
TG = 512
NB = TG // P
NG = TOK // TG
BPS = SEQ // P
NE = 32
NITEM = 2 * NE
ALPHA = float(2.0 ** 0.25)
EPS = 1e-5
GLU_A = 1.702
C7 = float(GLU_A * 7.0 / (1.0 + np.exp(-GLU_A * 7.0)))
NEG = -30000.0
TWO_PI = float(2.0 * np.pi)
CW1 = 6.28125
CW2 = float(2.0 * np.pi - 6.28125)
PI_LO = 3.1415925
NFM = 12 * P
NTM = 128 + 512 + 512
SLOT = 12288


def build_program(ng=NG, dbg=None):
    nc = bass.Bass("TRN2", target_bir_lowering=False)
    dram = lambda name, shape, dt, kind="ExternalInput": nc.dram_tensor(name, list(shape), dt, kind=kind).ap()
    x_d = dram("x", [TOK, D], F32)
    pos_d = dram("pos", [1, TOK], I32)
    ident_d = dram("ident", [P, P], F32)
    mask_d = dram("mask2", [P, 256], F32)
    tril_d = dram("tril", [P, P], F32)
    pvec_d = dram("pvec", [P, 2], F32)
    lnrows_d = dram("lnrows", [6, D], F32)
    wfm_d = dram("w_in_fm", [D, NFM], F32)
    wtm_d = dram("w_in_tm", [D, NTM], F32)
    wout_d = dram("w_out", [D, D], F32)
    bfm_d = dram("b_fm", [P, 12], F32)
    brow_d = dram("b_rows", [1, NTM + D], F32)
    sinks_d = dram("sinks", [1, 8], F32)
    gn_d = dram("gn", [2, 64], F32)
    wst_d = dram("wsT", [P, 8 * P], F32)
    bst_d = dram("bsT", [P, 8], F32)
    wr_d = dram("w_router", [D, NE], F32)
    br_d = dram("b_router", [1, NE], F32)
    ne_decl = NE if dbg is None else dbg.get("ne", NE)
    wgu_d = dram("w_gu", [ne_decl * 2 * D, D], F32)
    wd_d = dram("w_down", [ne_decl * D, D], F32)
    bgu_d = dram("b_gu", [P, NE * 16], F32)
    bdn_d = dram("b_down", [NE, D], F32)
    out_d = dram("out", [TOK, D], F32, kind="ExternalOutput")
    fmk_d = dram("fmk", [P, NG], F32)
    dbg_d = dram("dbg", [P, 4096], F32, kind="ExternalOutput") if dbg else None
    s_wfm = dram("s_wfm", [D, NFM], BF16, kind="Internal")
    s_wtm = dram("s_wtm", [D, NTM], BF16, kind="Internal")
    s_wout = dram("s_wout", [D, D], BF16, kind="Internal")
    s_gu = dram("s_gu", [ne_decl * 2 * D, D], BF16, kind="Internal")
    s_wd = dram("s_wd", [ne_decl * D, D], BF16, kind="Internal")

    S0, S1 = Sched(), Sched()
    cur = [S0]
    es = ExitStack()
    sb = lambda name, shape, dt: es.enter_context(nc.sbuf_tensor(name, list(shape), dt))
    id_f = sb("id_f", [P, P], F32)
    id_b = sb("id_b", [P, P], BF16)
    mask2 = sb("mask2s", [P, 256], F32)
    pvec = sb("pvecs", [P, 2], F32)
    epst = sb("epst", [P, 1], F32)
    fmk = sb("fmks", [P, 1], F32)
    lnbc = sb("lnbc", [P, 6, D], F32)
    sinkb = sb("sinkb", [P, 8], F32)
    negsink = sb("negsink", [P, 8], F32)
    gnbc = sb("gnbc", [P, 2, 64], F32)
    wsb = sb("wsb", [P, 8, P], BF16)
    bst = sb("bst", [P, 8], F32)
    wr = sb("wr", [P, KC, NE], F32)
    brt = sb("brt", [P, NE], F32)
    bdn = sb("bdn", [P, D], F32)
    bgu = sb("bgu", [P, NE, 16], F32)
    bfm = sb("bfm", [P, 12], F32)
    brow = sb("brow", [1, NTM + D], BF16)
    ones_b = sb("ones_b", [1, P], BF16)
    ones_f = sb("ones_f", [1, P], F32)
    XB = sb("XB", [P, NB, D], F32)
    hT = sb("hT", [P, KC, TG], BF16)
    G = sb("G", [P, NB, NE], F32)
    Gs = sb("Gs", [P, NB, NE], F32)
    xs = sb("xs", [P, D], F32)
    ost = [sb("ost%d" % i, [P, D], F32) for i in range(2)]
    h1Tf = sb("h1Tf", [P, KC, P], F32)
    lnst = sb("lnst", [P, 16], F32)
    posi = sb("posi", [P, P], I32)
    rp = sb("rp", [P, 8, P], F32)
    rpi = sb("rpi", [P, P], I32)
    t1s = sb("t1s", [P, 2, P], F32)
    t2s = sb("t2s", [P, 2, P], F32)
    qT = sb("qT", [P, 4, P], BF16)
    kT = sb("kT", [P, 2, 2, P], BF16)
    Vt = sb("Vt", [P, 2, P], BF16)
    sm = sb("sm", [P, 8, 256], F32)
    pn = sb("pn", [P, 8, 256], BF16)
    PTs = sb("PTs", [P, 8, 2, P], BF16)
    sst = sb("sst", [P, 6, 8], F32)
    OT = sb("OT", [P, 4, P], BF16)
    gua = sb("gua", [P, 512], F32)
    gva = sb("gva", [P, 512], F32)
    sq = sb("sq", [P, 512], F32)
    vn = sb("vn", [P, 512], BF16)
    gmix = sb("gmix", [P, 512], BF16)
    gmT = sb("gmT", [P, 4, P], BF16)
    gst = sb("gst", [P, 6, 8], F32)
    lg = sb("lg", [P, 3, NE], F32)
    rst = sb("rst", [P, 16], F32)
    GTs = sb("GTs", [P, P], F32)
    Gpad = sb("Gpad", [P, P], F32)
    sbuf_s = [sb("silu%d" % i, [P, 512], F32) for i in range(2)]
    sbuf_u = [sb("up%d" % i, [P, 512], F32) for i in range(2)]
    actT = [sb("actT%d" % i, [P, 4, 512], BF16) for i in range(2)]
    arena = [sb("arena%d" % i, [P, SLOT], BF16) for i in range(3)]
    pst = [es.enter_context(nc.psum_tensor("ps%d" % i, [P, 512], F32)) for i in range(8)]

    ps_ctr = [0]

    def psum():
        i = ps_ctr[0] % 8
        ps_ctr[0] += 1
        return pst[i], ("ps", i)

    def DVE(fn, r=(), w=()):
        return cur[0].op("dve", fn, r, w)

    def ACT(fn, r=(), w=()):
        return cur[0].op("act", fn, r, w)

    def POOL(fn, r=(), w=()):
        return cur[0].op("pool", fn, r, w)

    def PE(fn, r=(), w=()):
        return cur[0].op("pe", fn, r, w)

    def DMA(eng, out, in_, r=(), w=()):
        return cur[0].op(eng, lambda e: e.dma_start(out=out, in_=in_), r, w, dma=True)

    def DMAD(eng, fn, r=(), w=(), slow=False):
        def f(e, g):
            o_, i_ = fn(g)
            return e.dma_start(out=o_, in_=i_, allow_slow_non_contiguous=slow)
        return cur[0].op(eng, f, r, w, dma=True, dyn=True)

    def mm(out, lhsT, rhs, start, stop, r, w):
        return PE(lambda e: e.matmul(out, lhsT=lhsT, rhs=rhs, start=start, stop=stop), r, w)

    XBf = XB[:, :, :].rearrange("p a b -> p (a b)")

    DMA("sp", id_f[:, :], ident_d, w=["id_f"])
    DMA("sp", mask2[:, :], mask_d, w=["mask2"])
    DMA("sp", pvec[:, :], pvec_d, w=["pvec"])
    for i in range(6):
        DMA("sp", lnbc[:, i, :], lnrows_d[i:i + 1, :].partition_broadcast(P), w=["lnbc"])
    DMA("sp", sinkb[:, :], sinks_d[0:1, :].partition_broadcast(P), w=["sinkb"])
    for i in range(2):
        DMA("sp", gnbc[:, i, :], gn_d[i:i + 1, :].partition_broadcast(P), w=["gnbc"])
    DMA("sp", bst[:, :], bst_d, w=["bst"])
    DMA("sp", wr[:, :, :], wr_d.rearrange("(k p) n -> p k n", p=P), w=["wr"])
    DMA("sp", brt[:, :], br_d[0:1, :].partition_broadcast(P), w=["brt"])
    DVE(lambda e: e.memset(bdn[:, :], 0.0), w=["bdn"])
    DMA("sp", bdn[0:NE, :], bdn_d, w=["bdn"])
    DMA("sp", bgu[:, :, :].rearrange("p a b -> p (a b)"), bgu_d, w=["bgu"])
    DMA("sp", bfm[:, :], bfm_d, w=["bfm"])
    DMA("sp", XB[:, 0, :], wst_d, w=[("XB", 0)])
    DMA("sp", xs[:, 0:P], tril_d, w=["xs"])
    DMA("sp", XB[0:1, 1:4, :].rearrange("p a b -> p (a b)")[:, 0:NTM + D], brow_d, w=[("XB", 1), ("XB", 2), ("XB", 3)])
    DVE(lambda e: e.tensor_copy(out=id_b[:, :], in_=id_f[:, :]), r=["id_f"], w=["id_b"])
    DVE(lambda e: e.memset(epst[:, :], EPS), w=["epst"])
    DVE(lambda e: e.memset(Gpad[:, :], 0.0), w=["Gpad"])
    DVE(lambda e: e.memset(ones_b[:, :], 1.0), w=["ones_b"])
    DVE(lambda e: e.memset(ones_f[:, :], 1.0), w=["ones_f"])
    DVE(lambda e: e.tensor_scalar(out=negsink[:, :], in0=sinkb[:, :], scalar1=-1.0, scalar2=None, op0=ALU.mult),
        r=["sinkb"], w=["negsink"])
    DVE(lambda e: e.tensor_tensor(out=wsb[:, :, :], in0=XB[:, 0, :].rearrange("p (h t) -> p h t", h=8),
                                  in1=xs[:, 0:P].unsqueeze(1).to_broadcast([P, 8, P]), op=ALU.mult),
        r=[("XB", 0), "xs"], w=["wsb"])
    DVE(lambda e: e.tensor_copy(out=brow[:, :], in_=XB[0:1, 1:4, :].rearrange("p a b -> p (a b)")[:, 0:NTM + D]),
        r=[("XB", 1), ("XB", 2), ("XB", 3)], w=["brow"])
    DVE(lambda e: e.tensor_scalar(out=bgu[:, :, 0:8], in0=bgu[:, :, 0:8], scalar1=GLU_A, scalar2=None, op0=ALU.mult),
        r=["bgu"], w=["bgu"])
    DVE(lambda e: e.tensor_scalar(out=bgu[:, :, 8:16], in0=bgu[:, :, 8:16], scalar1=1.0, scalar2=None, op0=ALU.add),
        r=["bgu"], w=["bgu"])

    DVE(lambda e: e.memset(kT[:, :, :, :], 0.0), w=["kT0"])
    DVE(lambda e: e.memset(Vt[:, :, :], 0.0), w=["Vt0"])
    cv_ctr = [0]

    cv_in = [arena[j // 3][:, :].bitcast(F32)[:, (j % 3) * 2048:(j % 3) * 2048 + 2048] for j in range(6)]
    cv_out = [arena[2][:, j * 2048:(j + 1) * 2048] for j in range(4)] + \
             [hT[:, 4 * j:4 * j + 4, :].rearrange("p a b -> p (a b)") for j in range(2)]

    def convert(src, dst, total):
        W = 2048 if total % (P * 2048) == 0 else 1024
        sf = src.rearrange("a b -> (a b)")
        df = dst.rearrange("a b -> (a b)")
        for i in range(total // (P * W)):
            k = cv_ctr[0]
            cv_ctr[0] += 1
            stg = cv_in[k % 6][:, 0:W]
            stb = cv_out[k % 6][:, 0:W]
            ik = [("cvi", k % 6)]
            ok = [("cvo", k % 6)]
            DMA("sp", stg, sf[i * P * W:(i + 1) * P * W].rearrange("(p n) -> p n", p=P), w=ik)
            which = k % 5
            if which in (0, 2):
                DVE(lambda e, stg=stg, stb=stb: e.tensor_copy(out=stb, in_=stg), r=ik, w=ok)
            elif which in (1, 3):
                ACT(lambda e, stg=stg, stb=stb: e.activation(out=stb, in_=stg, func=AF.Identity), r=ik, w=ok)
            else:
                POOL(lambda e, stg=stg, stb=stb: e.tensor_copy(out=stb, in_=stg), r=ik, w=ok)
            DMA("act", df[i * P * W:(i + 1) * P * W].rearrange("(p n) -> p n", p=P), stb, r=ok)

    def convert_all():
        convert(wfm_d, s_wfm, D * NFM)
        convert(wtm_d, s_wtm, D * NTM)
        convert(wout_d, s_wout, D * D)
        convert(wgu_d, s_gu, ne_decl * 2 * D * D)
        convert(wd_d, s_wd, ne_decl * D * D)

    items = [("fm", 0), ("tm", 0), ("wo", 0)] + [("ex", 0, i) for i in range(NITEM)]
    arena_pos = [0]
    item_slot = {}

    def akeys(s_):
        return [("agu", s_), ("ad", s_)]

    def arena_load():
        k = arena_pos[0]
        arena_pos[0] += 1
        it = items[k]
        s_ = k % 3
        item_slot[it] = s_
        a = arena[s_]
        src, n = {"fm": (s_wfm, NFM), "tm": (s_wtm, NTM), "wo": (s_wout, D)}[it[0]]
        DMA("sp" if it[0] == "fm" else "act", a[:, 0:KC * n].rearrange("p (k n) -> p k n", k=KC), src.rearrange("(k p) n -> p k n", p=P), w=akeys(s_))

    def load_ex_gu(i):
        s_ = i % 3
        item_slot[("ex", 0, i)] = s_
        e_, hf = i // 2, i % 2
        r0 = (e_ * 2 + hf) * D
        DMA("sp", arena[s_][:, 0:KC * D].rearrange("p (k n) -> p k n", k=KC),
            s_gu[r0:r0 + D, :].rearrange("(k p) n -> p k n", p=P), w=[("agu", s_)])

    def load_ex_d(i):
        s_ = i % 3
        e_, hf = i // 2, i % 2
        r1 = e_ * D + hf * 512
        DMA("act", arena[s_][:, KC * D:KC * D + 4 * D].rearrange("p (m n) -> p m n", m=4),
            s_wd[r1:r1 + 512, :].rearrange("(m p) n -> p m n", p=P), w=[("ad", s_)])

    def layer_norm(src, dst, gi, rkeys, wkeys):
        for h in range(2):
            DVE(lambda e, h=h: e.bn_stats(out=lnst[:, 6 * h:6 * h + 6], in_=src[:, 512 * h:512 * h + 512]),
                r=rkeys, w=["lnst"])
        DVE(lambda e: e.bn_aggr(out=lnst[:, 12:14], in_=lnst[:, 0:12]), r=["lnst"], w=["lnmv"])
        ACT(lambda e: e.activation(out=lnst[:, 14:15], in_=lnst[:, 13:14], func=AF.Sqrt, bias=epst[:, 0:1], scale=1.0),
            r=["lnmv", "epst"], w=["lnsd"])
        DVE(lambda e: e.reciprocal(out=lnst[:, 15:16], in_=lnst[:, 14:15]), r=["lnsd"], w=["lnrs"])
        DVE(lambda e: e.tensor_scalar(out=dst, in0=src, scalar1=lnst[:, 12:13], scalar2=lnst[:, 15:16],
                                      op0=ALU.subtract, op1=ALU.mult), r=list(rkeys) + ["lnmv", "lnrs"], w=wkeys)
        DVE(lambda e: e.tensor_tensor(out=dst, in0=dst, in1=lnbc[:, gi, :], op=ALU.mult), r=list(wkeys) + ["lnbc"], w=wkeys)
        DVE(lambda e: e.tensor_tensor(out=dst, in0=dst, in1=lnbc[:, gi + 1, :], op=ALU.add), r=list(wkeys) + ["lnbc"], w=wkeys)

    def transpose_to_hT(b, also_f32):
        cols = slice(b * P, (b + 1) * P)
        for half in range(2):
            bank, bk = psum()
            for j in range(4):
                kc = half * 4 + j
                PE(lambda e, kc=kc, j=j, bank=bank: e.transpose(out=bank[:, j * P:(j + 1) * P],
                                                                 in_=XB[:, b, kc * P:(kc + 1) * P], identity=id_f[:, :]),
                   r=[("XB", b), "id_f"], w=[bk])
            ACT(lambda e, half=half, bank=bank: e.activation(
                out=hT[:, half * 4:half * 4 + 4, cols], in_=bank[:, :].rearrange("p (j t) -> p j t", j=4), func=AF.Identity),
                r=[bk], w=[("hT", b), ("evac", half)])
            if also_f32:
                DVE(lambda e, half=half, bank=bank: e.tensor_copy(
                    out=h1Tf[:, half * 4:half * 4 + 4, :], in_=bank[:, :].rearrange("p (j t) -> p j t", j=4)),
                    r=[bk, ("evac", half)], w=["h1Tf"])

    def load_x(b):
        DMAD("sp", lambda g, b=b: (xs[:, :], x_d[bass.ds(g * TG + b * P, P), :]), w=["xs"])

    def load_pos(b):
        DMAD("sp", lambda g, b=b: (posi[:, :], pos_d[0:1, bass.ds(g * TG + b * P, P)].partition_broadcast(P)), w=["posi"])

    def load_fmask():
        DMAD("sp", lambda g: (fmk[:, :], fmk_d[:, bass.ds(g, 1)]), w=["fmk"], slow=True)

    nblocks = NB
    stop_at = 9 if dbg is None else dbg.get("stop", 9)

    def mixer_block(g, b):
        gb = b
        first = False
        par = gb % 2
        cols = slice(b * P, (b + 1) * P)
        wfm = arena[item_slot[("fm", g)]][:, 0:KC * NFM].rearrange("p (k n) -> p k n", k=KC)
        wtm = arena[item_slot[("tm", g)]][:, 0:KC * NTM].rearrange("p (k n) -> p k n", k=KC)
        wo = arena[item_slot[("wo", g)]][:, 0:KC * D].rearrange("p (k n) -> p k n", k=KC)
        kfm = akeys(item_slot[("fm", g)])
        ktm = akeys(item_slot[("tm", g)])
        kwo = akeys(item_slot[("wo", g)])
        xbk = ("XB", b)
        hk = ("hT", b)

        layer_norm(xs[:, :], XB[:, b, :], 0, ["xs"], [xbk])
        if gb + 1 < nblocks:
            load_x(b + 1)
        transpose_to_hT(b, False)

        A_, U_, KF_, R_, C_, S_, U2_, KF2_ = [rp[:, i, :] for i in range(8)]
        DVE(lambda e: e.tensor_copy(out=A_, in_=posi[:, :]), r=["posi"], w=["rpA"])
        if gb + 1 < nblocks:
            load_pos(b + 1)
        DVE(lambda e: e.tensor_scalar(out=A_, in0=A_, scalar1=pvec[:, 0:1], scalar2=None, op0=ALU.mult), r=["rpA", "pvec"], w=["rpA"])
        DVE(lambda e: e.tensor_scalar(out=U_, in0=A_, scalar1=1.0 / TWO_PI, scalar2=None, op0=ALU.mult), r=["rpA"], w=["rpU"])
        DVE(lambda e: e.tensor_scalar(out=U2_, in0=U_, scalar1=0.25, scalar2=None, op0=ALU.add), r=["rpU"], w=["rpU2"])
        for (u_, kf_, ku, kk, shift, dst, kd) in ((U_, KF_, "rpU", "rpK", 0.0, S_, "rpS"), (U2_, KF2_, "rpU2", "rpK2", float(np.pi / 2), C_, "rpC")):
            DVE(lambda e, u_=u_: e.tensor_copy(out=rpi[:, :], in_=u_), r=[ku], w=["rpi"])
            DVE(lambda e, kf_=kf_: e.tensor_copy(out=kf_, in_=rpi[:, :]), r=["rpi"], w=[kk])
            DVE(lambda e, kf_=kf_: e.scalar_tensor_tensor(out=R_, in0=kf_, scalar=-CW1, in1=A_, op0=ALU.mult, op1=ALU.add),
                r=[kk, "rpA"], w=["rpR"])
            DVE(lambda e, kf_=kf_: e.scalar_tensor_tensor(out=R_, in0=kf_, scalar=-CW2, in1=R_, op0=ALU.mult, op1=ALU.add),
                r=[kk, "rpR"], w=["rpR"])
            if shift:
                DVE(lambda e, shift=shift: e.tensor_scalar(out=R_, in0=R_, scalar1=shift, scalar2=None, op0=ALU.add), r=["rpR"], w=["rpR"])
            DVE(lambda e: e.tensor_scalar(out=R_, in0=R_, scalar1=-PI_LO, scalar2=PI_LO, op0=ALU.max, op1=ALU.min), r=["rpR"], w=["rpR"])
            ACT(lambda e, dst=dst: e.activation(out=dst, in_=R_, func=AF.Sin), r=["rpR"], w=[kd])

        fb = []
        for j3 in range(3):
            bank, bk = psum()
            fb.append((bank, bk))
            for j in range(4):
                jj = j3 * 4 + j
                for kc in range(KC):
                    mm(bank[:, j * P:(j + 1) * P], wfm[:, kc, jj * P:(jj + 1) * P], hT[:, kc, cols], kc == 0, kc == KC - 1,
                       kfm + [hk], [bk])
        tb_ = [psum() for _ in range(3)]
        tsl = [(0, 128), (128, 640), (640, 1152)]
        for kc in range(KC):
            for (bank, bk), (c0, c1) in zip(tb_, tsl):
                mm(bank[:, 0:c1 - c0], hT[:, kc, cols], wtm[:, kc, c0:c1], kc == 0, False, ktm + [hk], [bk])
        for (bank, bk), (c0, c1) in zip(tb_, tsl):
            mm(bank[:, 0:c1 - c0], ones_b[0:1, :], brow[0:1, c0:c1], False, True, ["ones_b", "brow"], [bk])

        if stop_at <= 1:
            return
        for j in range(6):
            if j < 4:
                (qb, qk), qo = fb[0], j * P
                (sbk_, sk), so = fb[1], j * P
                bq, bs_ = j, 4 + j
                dst, dk = qT[:, j, :], "qT"
            else:
                (qb, qk), qo = fb[2], (j - 4) * P
                (sbk_, sk), so = fb[2], (j - 2) * P
                bq, bs_ = 8 + (j - 4), 10 + (j - 4)
                dst, dk = kT[:, par, j - 4, :], ("kT", par)
            DVE(lambda e, qb=qb, qo=qo, bq=bq, j=j: e.scalar_tensor_tensor(
                out=t1s[:, j % 2, :], in0=qb[:, qo:qo + P], scalar=bfm[:, bq:bq + 1], in1=C_, op0=ALU.add, op1=ALU.mult),
                r=[qk, "bfm", "rpC"], w=[("t1", j % 2)])
            DVE(lambda e, sbk_=sbk_, so=so, bs_=bs_, j=j: e.scalar_tensor_tensor(
                out=t2s[:, j % 2, :], in0=sbk_[:, so:so + P], scalar=bfm[:, bs_:bs_ + 1], in1=S_, op0=ALU.add, op1=ALU.mult),
                r=[sk, "bfm", "rpS"], w=[("t2", j % 2)])
            DVE(lambda e, dst=dst, j=j: e.scalar_tensor_tensor(
                out=dst, in0=t2s[:, j % 2, :], scalar=pvec[:, 1:2], in1=t1s[:, j % 2, :], op0=ALU.mult, op1=ALU.add),
                r=[("t1", j % 2), ("t2", j % 2), "pvec"], w=[dk])
        ACT(lambda e: e.activation(out=Vt[:, par, :], in_=tb_[0][0][:, 0:P], func=AF.Identity), r=[tb_[0][1]], w=[("Vt", par)])
        ACT(lambda e: e.activation(out=gua[:, :], in_=tb_[1][0][:, :], func=AF.Gelu), r=[tb_[1][1]], w=["gua"])
        ACT(lambda e: e.activation(out=gva[:, :], in_=tb_[2][0][:, :], func=AF.Gelu), r=[tb_[2][1]], w=["gva"])

        if stop_at <= 2:
            return
        koff = P if first else 0
        nk = 256 - koff
        scb = [psum() for _ in range(4)]
        SLOT_HEAD = (0, 2, 4, 6, 1, 3, 5, 7)
        for sl in range(8):
            hq = SLOT_HEAD[sl]
            c, half, kvg = hq // 2, hq % 2, hq // 4
            bank, bk = scb[sl // 2]
            off = (sl % 2) * 256
            prt = slice(half * 64, half * 64 + 64)
            if not first:
                mm(bank[:, off:off + P], qT[prt, c, :], kT[prt, 1 - par, kvg, :], True, True, ["qT", ("kT", 1 - par)], [bk])
            mm(bank[:, off + P:off + 256], qT[prt, c, :], kT[prt, par, kvg, :], True, True, ["qT", ("kT", par)], [bk])
        for j in range(4):
            bank, bk = scb[j]
            DVE(lambda e, bank=bank, j=j: e.scalar_tensor_tensor(
                out=sm[:, 2 * j:2 * j + 2, koff:256], in0=bank[:, :].rearrange("p (h k) -> p h k", h=2)[:, :, koff:256],
                scalar=0.125, in1=mask2[:, koff:256].unsqueeze(1).to_broadcast([P, 2, nk]), op0=ALU.mult, op1=ALU.add),
                r=[bk, "mask2"], w=[("sm", j)])
        smk = [("sm", j) for j in range(4)]
        if b == 0:
            DVE(lambda e: e.tensor_scalar(out=sm[:, :, 0:P], in0=sm[:, :, 0:P], scalar1=fmk[:, 0:1], scalar2=None, op0=ALU.add),
                r=smk + ["fmk"], w=smk)
        for j in range(4):
            DVE(lambda e, j=j: e.tensor_reduce(out=sst[:, 0, 2 * j:2 * j + 2], in_=sm[:, 2 * j:2 * j + 2, koff:256], axis=AX.X, op=ALU.max),
                r=[("sm", j)], w=[("mx", j)])
        DVE(lambda e: e.scalar_tensor_tensor(out=sst[:, 1, :], in0=sst[:, 0, :], scalar=-1.0, in1=negsink[:, :], op0=ALU.mult, op1=ALU.min),
            r=[("mx", j) for j in range(4)] + ["negsink"], w=["negm"])
        for sl in range(8):
            ACT(lambda e, sl=sl: e.activation(out=sm[:, sl, koff:256], in_=sm[:, sl, koff:256], func=AF.Exp,
                                              bias=sst[:, 1, sl:sl + 1], scale=1.0),
                r=[("sm", sl // 2), "negm"], w=[("sm", sl // 2)])
        DVE(lambda e: e.tensor_reduce(out=sst[:, 2, :], in_=sm[:, :, koff:256], axis=AX.X, op=ALU.add), r=smk, w=["rs"])
        DVE(lambda e: e.tensor_tensor(out=sst[:, 3, :], in0=sst[:, 1, :], in1=sinkb[:, :], op=ALU.add), r=["negm", "sinkb"], w=["es"])
        ACT(lambda e: e.activation(out=sst[:, 3, :], in_=sst[:, 3, :], func=AF.Exp), r=["es"], w=["es"])
        DVE(lambda e: e.tensor_tensor(out=sst[:, 4, :], in0=sst[:, 2, :], in1=sst[:, 3, :], op=ALU.add), r=["rs", "es"], w=["den"])
        DVE(lambda e: e.reciprocal(out=sst[:, 5, :], in_=sst[:, 4, :]), r=["den"], w=["rcp"])
        DVE(lambda e: e.tensor_tensor(out=pn[:, :, koff:256], in0=sm[:, :, koff:256],
                                      in1=sst[:, 5, :].unsqueeze(2).to_broadcast([P, 8, nk]), op=ALU.mult),
            r=smk + ["rcp"], w=["pn"])
        ptb = [psum() for _ in range(2)]
        for hq in range(8):
            bank, bk = ptb[hq // 4]
            bv = bank[:, :].bitcast(BF16)
            for kb in range(2):
                if first and kb == 0:
                    continue
                o0 = ((hq % 4) * 2 + kb) * P
                PE(lambda e, bv=bv, o0=o0, hq=hq, kb=kb: e.transpose(out=bv[:, o0:o0 + P], in_=pn[:, hq, kb * P:(kb + 1) * P], identity=id_b[:, :]),
                   r=["pn", "id_b"], w=[bk])
        k0 = 1 if first else 0
        for j in range(2):
            bank, bk = ptb[j]
            bv = bank[:, :].bitcast(BF16).rearrange("p (h k t) -> p h k t", h=4, k=2)
            ACT(lambda e, bv=bv, j=j: e.activation(out=PTs[:, 4 * j:4 * j + 4, k0:2, :], in_=bv[:, :, k0:2, :], func=AF.Identity),
                r=[bk], w=[("PTs", j)])
        ob, obk = psum()
        for sl in range(8):
            hq = SLOT_HEAD[sl]
            c, half, kvg = hq // 2, hq % 2, hq // 4
            o_ap = ob[half * 64:half * 64 + 64, c * P:(c + 1) * P]
            if not first:
                mm(o_ap, Vt[:, 1 - par, kvg * 64:kvg * 64 + 64], PTs[:, sl, 0, :], True, False, [("Vt", 1 - par), ("PTs", sl // 4)], [obk])
            mm(o_ap, Vt[:, par, kvg * 64:kvg * 64 + 64], PTs[:, sl, 1, :], first, True, [("Vt", par), ("PTs", sl // 4)], [obk])
        ACT(lambda e: e.activation(out=OT[:, :, :], in_=ob[:, :].rearrange("p (c t) -> p c t", c=4), func=AF.Identity), r=[obk], w=["OT"])

        if stop_at <= 3:
            return
        gva3 = gva[:, :].rearrange("p (h d) -> p h d", h=8)
        sq3 = sq[:, :].rearrange("p (h d) -> p h d", h=8)
        DVE(lambda e: e.tensor_reduce(out=gst[:, 0, :], in_=gva3, axis=AX.X, op=ALU.add), r=["gva"], w=["gs1"])
        ACT(lambda e: e.activation(out=sq[:, :], in_=gva[:, :], func=AF.Square), r=["gva"], w=["sq"])
        DVE(lambda e: e.tensor_reduce(out=gst[:, 1, :], in_=sq3, axis=AX.X, op=ALU.add), r=["sq"], w=["gs2"])
        DVE(lambda e: e.tensor_scalar(out=gst[:, 2, :], in0=gst[:, 0, :], scalar1=1.0 / 64, scalar2=None, op0=ALU.mult), r=["gs1"], w=["gmean"])
        DVE(lambda e: e.tensor_tensor(out=gst[:, 3, :], in0=gst[:, 2, :], in1=gst[:, 2, :], op=ALU.mult), r=["gmean"], w=["gmsq"])
        DVE(lambda e: e.scalar_tensor_tensor(out=gst[:, 4, :], in0=gst[:, 1, :], scalar=1.0 / 64, in1=gst[:, 3, :], op0=ALU.mult, op1=ALU.subtract),
            r=["gs2", "gmsq"], w=["gvar"])
        ACT(lambda e: e.activation(out=gst[:, 4, :], in_=gst[:, 4, :], func=AF.Sqrt, bias=epst[:, 0:1], scale=1.0), r=["gvar", "epst"], w=["gvar"])
        DVE(lambda e: e.reciprocal(out=gst[:, 5, :], in_=gst[:, 4, :]), r=["gvar"], w=["grstd"])
        DVE(lambda e: e.tensor_tensor(out=gva3, in0=gva3, in1=gst[:, 2, :].unsqueeze(2).to_broadcast([P, 8, 64]), op=ALU.subtract),
            r=["gva", "gmean"], w=["gva"])
        DVE(lambda e: e.tensor_tensor(out=gva3, in0=gva3, in1=gst[:, 5, :].unsqueeze(2).to_broadcast([P, 8, 64]), op=ALU.mult),
            r=["gva", "grstd"], w=["gva"])
        DVE(lambda e: e.tensor_tensor(out=gva3, in0=gva3, in1=gnbc[:, 0, :].unsqueeze(1).to_broadcast([P, 8, 64]), op=ALU.mult),
            r=["gva", "gnbc"], w=["gva"])
        DVE(lambda e: e.tensor_tensor(out=vn[:, :].rearrange("p (h d) -> p h d", h=8), in0=gva3,
                                      in1=gnbc[:, 1, :].unsqueeze(1).to_broadcast([P, 8, 64]), op=ALU.add),
            r=["gva", "gnbc"], w=["vn"])
        spb, spk = psum()
        for h in range(8):
            mm(spb[:, h * 64:h * 64 + 64], wsb[:, h, :], vn[:, h * 64:h * 64 + 64], True, True, ["wsb", "vn"], [spk])
        DVE(lambda e: e.tensor_tensor(out=sq3, in0=spb[:, :].rearrange("p (h d) -> p h d", h=8),
                                      in1=bst[:, :].unsqueeze(2).to_broadcast([P, 8, 64]), op=ALU.add), r=[spk, "bst"], w=["sq"])
        DVE(lambda e: e.tensor_tensor(out=gmix[:, :], in0=sq[:, :], in1=gua[:, :], op=ALU.mult), r=["sq", "gua"], w=["gmix"])
        gtb, gtk = psum()
        gtv = gtb[:, :].bitcast(BF16)
        for c in range(4):
            PE(lambda e, c=c: e.transpose(out=gtv[:, c * P:(c + 1) * P], in_=gmix[:, c * P:(c + 1) * P], identity=id_b[:, :]),
               r=["gmix", "id_b"], w=[gtk])
        ACT(lambda e: e.activation(out=gmT[:, :, :], in_=gtv[:, 0:4 * P].rearrange("p (c t) -> p c t", c=4), func=AF.Identity), r=[gtk], w=["gmT"])

        if stop_at <= 4:
            return
        for half in range(2):
            bank, bk = psum()
            hs = slice(half * 512, half * 512 + 512)
            for c in range(4):
                mm(bank[:, :], OT[:, c, :], wo[:, c, hs], c == 0, False, ["OT"] + kwo, [bk])
            for c in range(4):
                mm(bank[:, :], gmT[:, c, :], wo[:, 4 + c, hs], False, False, ["gmT"] + kwo, [bk])
            mm(bank[:, :], ones_b[0:1, :], brow[0:1, NTM + half * 512:NTM + half * 512 + 512], False, True, ["ones_b", "brow"], [bk])
            DVE(lambda e, bank=bank, hs=hs: e.scalar_tensor_tensor(out=XB[:, b, hs], in0=XB[:, b, hs], scalar=ALPHA, in1=bank[:, :],
                                                                 op0=ALU.mult, op1=ALU.add), r=[bk, xbk], w=[xbk])
        layer_norm(XB[:, b, :], XB[:, b, :], 2, [xbk], [xbk])
        transpose_to_hT(b, True)

        if stop_at <= 5:
            return
        rb, rk = psum()
        for kc in range(KC):
            mm(rb[:, 0:NE], h1Tf[:, kc, :], wr[:, kc, :], kc == 0, kc == KC - 1, ["h1Tf", "wr"], [rk])
        DVE(lambda e: e.tensor_tensor(out=lg[:, 0, :], in0=rb[:, 0:NE], in1=brt[:, :], op=ALU.add), r=[rk, "brt"], w=["lg0"])
        DVE(lambda e: e.max(out=rst[:, 0:8], in_=lg[:, 0, :]), r=["lg0"], w=["mx8"])
        DVE(lambda e: e.tensor_scalar(out=lg[:, 1, :], in0=lg[:, 0, :], scalar1=rst[:, 3:4], scalar2=None, op0=ALU.is_ge), r=["lg0", "mx8"], w=["lg1"])
        DVE(lambda e: e.tensor_scalar(out=rst[:, 8:9], in0=rst[:, 0:1], scalar1=-1.0, scalar2=None, op0=ALU.mult), r=["mx8"], w=["nm1"])
        ACT(lambda e: e.activation(out=lg[:, 2, :], in_=lg[:, 0, :], func=AF.Exp, bias=rst[:, 8:9], scale=1.0), r=["lg0", "nm1"], w=["lg2"])
        DVE(lambda e: e.tensor_tensor(out=lg[:, 2, :], in0=lg[:, 2, :], in1=lg[:, 1, :], op=ALU.mult), r=["lg2", "lg1"], w=["lg2"])
        DVE(lambda e: e.tensor_reduce(out=rst[:, 9:10], in_=lg[:, 2, :], axis=AX.X, op=ALU.add), r=["lg2"], w=["ssum"])
        DVE(lambda e: e.reciprocal(out=rst[:, 10:11], in_=rst[:, 9:10]), r=["ssum"], w=["rc"])
        DVE(lambda e: e.tensor_scalar(out=Gpad[:, 0:NE], in0=lg[:, 2, :], scalar1=rst[:, 10:11], scalar2=None, op0=ALU.mult), r=["lg2", "rc"], w=["Gpad"])
        DVE(lambda e: e.tensor_copy(out=G[:, b, :], in_=Gpad[:, 0:NE]), r=["Gpad"], w=[("G", b)])
        DVE(lambda e: e.tensor_scalar(out=Gs[:, b, :], in0=Gpad[:, 0:NE], scalar1=1.0 / GLU_A, scalar2=None, op0=ALU.mult), r=["Gpad"], w=[("Gs", b)])
        gb_, gk_ = psum()
        PE(lambda e: e.transpose(out=gb_[:, 0:P], in_=Gpad[:, :], identity=id_f[:, :]), r=["Gpad", "id_f"], w=[gk_])
        ACT(lambda e: e.activation(out=GTs[:, :], in_=gb_[:, 0:P], func=AF.Identity), r=[gk_], w=["GTs"])
        for half in range(2):
            bank, bk = psum()
            hs = slice(half * 512, half * 512 + 512)
            mm(bank[:, :], GTs[:, :], bdn[:, hs], True, True, ["GTs", "bdn"], [bk])
            DVE(lambda e, bank=bank, hs=hs: e.scalar_tensor_tensor(out=XB[:, b, hs], in0=XB[:, b, hs], scalar=ALPHA, in1=bank[:, :],
                                                                 op0=ALU.mult, op1=ALU.add), r=[bk, xbk], w=[xbk])

    hkeys = [("hT", b) for b in range(NB)]
    nch = TG // 512

    def moe_gu(g, i):
        e_, hf = i // 2, i % 2
        s = item_slot[("ex", g, i)]
        ak = ("agu", s)
        wgu = arena[s][:, 0:KC * D].rearrange("p (k n) -> p k n", k=KC)
        for ch in range(nch):
            tk = slice(ch * 512, ch * 512 + 512)
            ab = actT[(i * nch + ch) % 2]
            abk = ("actT", (i * nch + ch) % 2)
            for m in range(4):
                pg, pgk = psum()
                pu, puk = psum()
                for kc in range(KC):
                    mm(pg[:, :], wgu[:, kc, m * P:(m + 1) * P], hT[:, kc, tk], kc == 0, kc == KC - 1, [ak] + hkeys[ch * 4:ch * 4 + 4], [pgk])
                for kc in range(KC):
                    mm(pu[:, :], wgu[:, kc, 512 + m * P:512 + (m + 1) * P], hT[:, kc, tk], kc == 0, kc == KC - 1, [ak] + hkeys[ch * 4:ch * 4 + 4], [puk])
                j = hf * 4 + m
                sbi = (m % 2)
                sS, sU = sbuf_s[sbi], sbuf_u[sbi]
                ACT(lambda e, pg=pg, sS=sS, j=j: e.activation(out=sS[:, :], in_=pg[:, :], func=AF.Silu, bias=bgu[:, e_, j:j + 1], scale=GLU_A),
                    r=[pgk, "bgu"], w=[("sS", sbi)])
                ACT(lambda e, pu=pu, sU=sU, j=j: e.activation(out=sU[:, :], in_=pu[:, :], func=AF.Identity, bias=bgu[:, e_, 8 + j:9 + j], scale=1.0),
                    r=[puk, "bgu"], w=[("sU", sbi)])
                DVE(lambda e, sU=sU: e.tensor_scalar(out=sU[:, :], in0=sU[:, :], scalar1=-6.0, scalar2=8.0, op0=ALU.max, op1=ALU.min),
                    r=[("sU", sbi)], w=[("sU", sbi)])
                DVE(lambda e, sS=sS, sU=sU, ab=ab, m=m: e.scalar_tensor_tensor(out=ab[:, m, :], in0=sS[:, :], scalar=C7, in1=sU[:, :],
                                                                              op0=ALU.min, op1=ALU.mult),
                    r=[("sS", sbi), ("sU", sbi)], w=[abk])

    def moe_down(g, i):
        e_, hf = i // 2, i % 2
        s = item_slot[("ex", g, i)]
        ak = ("ad", s)
        wdn = arena[s][:, KC * D:KC * D + 4 * D].rearrange("p (m n) -> p m n", m=4)
        for ch in range(nch):
            ab = actT[(i * nch + ch) % 2]
            abk = ("actT", (i * nch + ch) % 2)
            for tbk in range(4):
                b = ch * 4 + tbk
                for half in range(2):
                    hs = slice(half * 512, half * 512 + 512)
                    pd, pdk = psum()
                    for m in range(4):
                        mm(pd[:, :], ab[:, m, tbk * P:(tbk + 1) * P], wdn[:, m, hs], m == 0, m == 3, [abk, ak], [pdk])
                    DVE(lambda e, pd=pd, hs=hs, b=b: e.scalar_tensor_tensor(out=XB[:, b, hs], in0=pd[:, :], scalar=Gs[:, b, e_:e_ + 1],
                                                                          in1=XB[:, b, hs], op0=ALU.mult, op1=ALU.add),
                        r=[pdk, ("Gs", b), ("XB", b)], w=[("XB", b)])

    def ln2_out(g):
        for b in range(NB):
            o = ost[b % 2]
            ok = ("ost", b % 2)
            layer_norm(XB[:, b, :], o[:, :], 4, [("XB", b)], [ok])
            DMAD("sp", lambda g, b=b, o=o: (out_d[bass.ds(g * TG + b * P, P), :], o[:, :]), r=[ok])

    stage = 9 if dbg is None else dbg.get("stage", 9)
    convert_all()
    cur[0] = S1
    load_x(0)
    load_pos(0)
    load_fmask()
    arena_load(); arena_load(); arena_load()
    g = 0
    if stage >= 1:
        for b in range(NB if dbg is None else dbg.get("nb", NB)):
            mixer_block(g, b)
    if stage >= 2:
        nit = NITEM if dbg is None else dbg.get("nitem", NITEM)
        for i in range(min(3, nit)):
            load_ex_gu(i)
            load_ex_d(i)
        moe_gu(g, 0)
        if 3 < nit:
            load_ex_gu(3)
        for i in range(nit):
            if i + 1 < nit:
                moe_gu(g, i + 1)
                if i + 4 < nit:
                    load_ex_gu(i + 4)
            moe_down(g, i)
            if i + 3 < nit:
                load_ex_d(i + 3)
    if stage >= 3:
        ln2_out(g)
    if dbg and dbg.get("fn"):
        dbg["fn"](cur[0], DMA, dbg_d, locals())

    f0 = S0.finalize()
    f1 = S1.finalize()
    sems = {}
    for nm in sorted(set(f0) | set(f1)):
        sems[nm] = es.enter_context(nc.semaphore("s_%s_%s_%d" % nm))
    bar_a = es.enter_context(nc.semaphore("bar_a"))
    bar_b = es.enter_context(nc.semaphore("bar_b"))

    def emit_engine(name, h):
        S0.emit(name, h, sems)
        S0.barrier(name, h, sems, bar_a, bar_b, 5)
        with h.Fori(0, ng) as gi:
            S1.emit(name, h, sems, gi)
            S1.barrier(name, h, sems, bar_a, bar_b, gi * 5 + 10)

    with nc.Block() as block:
        @block.tensor
        def _(e):
            emit_engine("pe", e)

        @block.vector
        def _(e):
            emit_engine("dve", e)

        @block.scalar
        def _(e):
            emit_engine("act", e)

        @block.gpsimd
        def _(e):
            emit_engine("pool", e)

        @block.sync
        def _(e):
            emit_engine("sp", e)
    es.close()
    return nc


def _partner(ch):
    return ch + 8 if ch < 8 else (ch - 8 if ch < 16 else ch)


def _host_inputs(inp):
    f = np.float32
    w_in = np.asarray(inp["w_in"], f)[0]
    b_in = np.asarray(inp["b_in"], f)[0]
    q_cols = np.arange(512)
    qs_cols = np.array([(c // 64) * 64 + _partner(c % 64) for c in range(512)])
    k0 = 512 + np.arange(64)
    k1 = 576 + np.arange(64)
    k0s = 512 + np.array([_partner(c) for c in range(64)])
    k1s = 576 + np.array([_partner(c) for c in range(64)])
    fm_cols = np.concatenate([q_cols, qs_cols, k0, k0, k1, k1, k0s, k0s, k1s, k1s])
    tm_cols = np.arange(640, 1792)
    inv_freq = (f(500000.0) ** (-np.arange(0, 16, 2, dtype=f) / f(16))).astype(f)
    pvec = np.zeros((P, 2), f)
    for p in range(P):
        ch = p % 64
        if ch < 16:
            pvec[p, 0] = inv_freq[ch % 8]
            pvec[p, 1] = -1.0 if ch < 8 else 1.0
    qi = np.arange(P)[:, None]
    kj = np.arange(P)[None, :]
    mask2 = np.concatenate([np.where(kj > qi, 0.0, NEG), np.where(kj <= qi, 0.0, NEG)], axis=1).astype(f)
    tril = (kj >= qi).astype(f)
    wgu = np.asarray(inp["w_gate_up"], f)[0].reshape(NE, D, 2, 2, 512).transpose(0, 3, 1, 2, 4)
    shared = {
        "ident": np.eye(P, dtype=f),
        "mask2": mask2,
        "tril": tril,
        "pvec": pvec,
        "fmk": np.ascontiguousarray(np.broadcast_to(np.where(np.arange(NG) % (SEQ // TG) == 0, NEG, 0.0).astype(f)[None, :], (P, NG))),
        "lnrows": np.stack([np.asarray(inp["ln_in_g"], f), np.asarray(inp["ln_in_b"], f),
                            np.asarray(inp["ln1_g"], f)[0], np.asarray(inp["ln1_b"], f)[0],
                            np.asarray(inp["ln2_g"], f)[0], np.asarray(inp["ln2_b"], f)[0]]),
        "w_in_fm": np.ascontiguousarray(w_in[:, fm_cols]),
        "w_in_tm": np.ascontiguousarray(w_in[:, tm_cols]),
        "w_out": np.ascontiguousarray(np.asarray(inp["w_out"], f)[0]),
        "b_fm": np.ascontiguousarray(b_in[fm_cols].reshape(12, P).T),
        "b_rows": np.concatenate([b_in[tm_cols], np.asarray(inp["b_out"], f)[0]])[None, :],
        "sinks": np.ascontiguousarray(np.asarray(inp["sinks"], f).reshape(8)[[0, 2, 4, 6, 1, 3, 5, 7]]).reshape(1, 8),
        "gn": np.stack([np.asarray(inp["gn_g"], f)[0], np.asarray(inp["gn_b"], f)[0]]),
        "wsT": np.ascontiguousarray(np.asarray(inp["w_s"], f)[0].transpose(2, 0, 1)).reshape(P, 8 * P),
        "bsT": np.ascontiguousarray(np.asarray(inp["b_s"], f)[0].T),
        "w_router": np.ascontiguousarray(np.asarray(inp["w_router"], f)[0]),
        "b_router": np.asarray(inp["b_router"], f).reshape(1, NE),
        "w_gu": np.ascontiguousarray(wgu).reshape(NE * 2 * D, D),
        "w_down": np.ascontiguousarray(np.asarray(inp["w_down"], f)[0]).reshape(NE * D, D),
        "b_gu": np.ascontiguousarray(np.asarray(inp["b_gate_up"], f)[0].reshape(NE, 16, P).transpose(2, 0, 1)).reshape(P, NE * 16),
        "b_down": np.ascontiguousarray(np.asarray(inp["b_down"], f)[0]),
    }
    x = np.asarray(inp["x"], f).reshape(-1, D)
    pos = np.asarray(inp["positions"], np.int32).reshape(1, -1)
    maps = []
    for c in range(8):
        m = dict(shared)
        m["x"] = np.ascontiguousarray(x[c * TOK:(c + 1) * TOK])
        m["pos"] = np.ascontiguousarray(pos[:, c * TOK:(c + 1) * TOK])
        maps.append(m)
    return maps


def kernel(**inputs):
    ng = int(inputs.pop("_ng", NG))
    maps = _host_inputs(inputs)
    nc = build_program(ng=ng)
    res = run_bass_kernel_spmd(nc, maps, core_ids=list(range(8)))
    out = np.concatenate([np.asarray(r["out"], np.float32) for r in res.results], axis=0)
    return out.reshape(16, SEQ, D)
```

```python
import numpy as np
from contextlib import ExitStack
import concourse.bass as bass
import concourse.mybir as mybir
from concourse.bass_utils import run_bass_kernel_spmd

F32, BF16, I32 = mybir.dt.float32, mybir.dt.bfloat16, mybir.dt.int32
ALU = mybir.AluOpType
AF = mybir.ActivationFunctionType
AX = mybir.AxisListType

ENGS = ("pe", "dve", "act", "pool", "sp")
SEM_CHUNK = 12000
DMA_POOL = {"sp": 12, "pool": 2, "act": 8}


class Tk:
    __slots__ = ("eng", "dma", "needed", "sem", "val", "pos")


class Sched:
    def __init__(self):
        self.ops = {e: [] for e in ENGS}
        self.lastw = {}
        self.readers = {}

    def op(self, eng, fn, r=(), w=(), dma=False, extra=(), dyn=False):
        t = Tk()
        t.eng, t.dma, t.needed, t.sem, t.val = eng, dma, dma, None, None
        t.pos = len(self.ops[eng])
        waits = []

        def consider(x, writer):
            if x is None:
                return
            if not x.dma and x.eng == eng and (eng == "pe" or not writer):
                return
            waits.append(x)

        for k in r:
            consider(self.lastw.get(k), True)
        for k in w:
            consider(self.lastw.get(k), True)
            rd = self.readers.get(k)
            if rd:
                for y in rd[0].values():
                    consider(y, False)
                for y in rd[1]:
                    consider(y, False)
        for x in extra:
            consider(x, True)
        for x in waits:
            x.needed = True
        self.ops[eng].append((fn, waits, t, dyn))
        for k in r:
            rd = self.readers.setdefault(k, ({}, []))
            if dma:
                rd[1].append(t)
            else:
                rd[0][eng] = t
        for k in w:
            self.lastw[k] = t
            self.readers[k] = ({}, [])
        return t

    def finalize(self):
        self.final = {}
        for e in ENGS:
            n = 0
            nd = 0
            for (_fn, _waits, t, _dyn) in self.ops[e]:
                if t.dma:
                    R = DMA_POOL[e]
                    t.sem = ("d", e, nd % R)
                    t.val = 16 * (nd // R + 1)
                    nd += 1
                elif t.needed:
                    t.sem = ("c", e, n // SEM_CHUNK)
                    t.val = n % SEM_CHUNK + 1
                    n += 1
                if t.sem is not None:
                    self.final[t.sem] = max(self.final.get(t.sem, 0), t.val)
        return self.final

    def emit(self, eng, h, sems, g=None):
        waited = {}

        def do_wait(sem, val):
            if waited.get(sem, 0) >= val:
                return
            waited[sem] = val
            h.wait_ge(sems[sem], val)

        for (fn, waits, t, dyn) in self.ops[eng]:
            need = {}
            for x in waits:
                if need.get(x.sem, 0) < x.val:
                    need[x.sem] = x.val
            if t.dma and t.val > 16:
                if need.get(t.sem, 0) < t.val - 16:
                    need[t.sem] = t.val - 16
            for sem in sorted(need):
                do_wait(sem, need[sem])
            ins = fn(h, g) if dyn else fn(h)
            if t.sem is not None:
                ins.then_inc(sems[t.sem], 16 if t.dma else 1)

    def barrier(self, eng, h, sems, bar_a, bar_b, target):
        for sem in sorted(self.final):
            h.wait_ge(sems[sem], self.final[sem])
        h.nop().then_inc(bar_a, 1)
        h.wait_ge(bar_a, target)
        for sem in sorted(self.final):
            if sem[1] == eng:
                h.sem_clear(sems[sem])
        h.nop().then_inc(bar_b, 1)
        h.wait_ge(bar_b, target)


P = 128
D = 1024
KC = 8
SEQ = 4096
TOK = 8192
TG = 512
NB = TG // P
NG = TOK // TG
BPS = SEQ // P
NE = 32
NITEM = 2 * NE
ALPHA = float(2.0 ** 0.25)
EPS = 1e-5
GLU_A = 1.702
C7 = float(GLU_A * 7.0 / (1.0 + np.exp(-GLU_A * 7.0)))
NEG = -30000.0
TWO_PI = float(2.0 * np.pi)
CW1 = 6.28125
CW2 = float(2.0 * np.pi - 6.28125)
PI_LO = 3.1415925
NFM = 12 * P
NTM = 128 + 512 + 512
SLOT = 12288


def build_program(ng=NG, dbg=None):
    nc = bass.Bass("TRN2", target_bir_lowering=False)
    dram = lambda name, shape, dt, kind="ExternalInput": nc.dram_tensor(name, list(shape), dt, kind=kind).ap()
    x_d = dram("x", [TOK, D], F32)
    pos_d = dram("pos", [1, TOK], I32)
    ident_d = dram("ident", [P, P], F32)
    mask_d = dram("mask2", [P, 256], F32)
    tril_d = dram("tril", [P, P], F32)
    pvec_d = dram("pvec", [P, 2], F32)
    lnrows_d = dram("lnrows", [6, D], F32)
    wfm_d = dram("w_in_fm", [D, NFM], F32)
    wtm_d = dram("w_in_tm", [D, NTM], F32)
    wout_d = dram("w_out", [D, D], F32)
    bfm_d = dram("b_fm", [P, 12], F32)
    brow_d = dram("b_rows", [1, NTM + D], F32)
    sinks_d = dram("sinks", [1, 8], F32)
    gn_d = dram("gn", [2, 64], F32)
    wst_d = dram("wsT", [P, 8 * P], F32)
    bst_d = dram("bsT", [P, 8], F32)
    wr_d = dram("w_router", [D, NE], F32)
    br_d = dram("b_router", [1, NE], F32)
    ne_decl = NE if dbg is None else dbg.get("ne", NE)
    wgu_d = dram("w_gu", [ne_decl * 2 * D, D], F32)
    wd_d = dram("w_down", [ne_decl * D, D], F32)
    bgu_d = dram("b_gu", [P, NE * 16], F32)
    bdn_d = dram("b_down", [NE, D], F32)
    out_d = dram("out", [TOK, D], F32, kind="ExternalOutput")
    fmk_d = dram("fmk", [P, NG], F32)
    dbg_d = dram("dbg", [P, 4096], F32, kind="ExternalOutput") if dbg else None
    s_wfm = dram("s_wfm", [D, NFM], BF16, kind="Internal")
    s_wtm = dram("s_wtm", [D, NTM], BF16, kind="Internal")
    s_wout = dram("s_wout", [D, D], BF16, kind="Internal")
    s_gu = dram("s_gu", [ne_decl * 2 * D, D], BF16, kind="Internal")
    s_wd = dram("s_wd", [ne_decl * D, D], BF16, kind="Internal")

    S0, S1 = Sched(), Sched()
    cur = [S0]
    es = ExitStack()
    sb = lambda name, shape, dt: es.enter_context(nc.sbuf_tensor(name, list(shape), dt))
    id_f = sb("id_f", [P, P], F32)
    id_b = sb("id_b", [P, P], BF16)
    mask2 = sb("mask2s", [P, 256], F32)
    pvec = sb("pvecs", [P, 2], F32)
    epst = sb("epst", [P, 1], F32)
    fmk = sb("fmks", [P, 1], F32)
    lnbc = sb("lnbc", [P, 6, D], F32)
    sinkb = sb("sinkb", [P, 8], F32)
    negsink = sb("negsink", [P, 8], F32)
    gnbc = sb("gnbc", [P, 2, 64], F32)
    wsb = sb("wsb", [P, 8, P], BF16)
    bst = sb("bst", [P, 8], F32)
    wr = sb("wr", [P, KC, NE], F32)
    brt = sb("brt", [P, NE], F32)
    bdn = sb("bdn", [P, D], F32)
    bgu = sb("bgu", [P, NE, 16], F32)
    bfm = sb("bfm", [P, 12], F32)
    brow = sb("brow", [1, NTM + D], BF16)
    ones_b = sb("ones_b", [1, P], BF16)
    ones_f = sb("ones_f", [1, P], F32)
    XB = sb("XB", [P, NB, D], F32)
    hT = sb("hT", [P, KC, TG], BF16)
    G = sb("G", [P, NB, NE], F32)
    Gs = sb("Gs", [P, NB, NE], F32)
    xs = sb("xs", [P, D], F32)
    ost = [sb("ost%d" % i, [P, D], F32) for i in range(2)]
    h1Tf = sb("h1Tf", [P, KC, P], F32)
    lnst = sb("lnst", [P, 16], F32)
    posi = sb("posi", [P, P], I32)
    rp = sb("rp", [P, 8, P], F32)
    rpi = sb("rpi", [P, P], I32)
    t1s = sb("t1s", [P, 2, P], F32)
    t2s = sb("t2s", [P, 2, P], F32)
    qT = sb("qT", [P, 4, P], BF16)
    kT = sb("kT", [P, 2, 2, P], BF16)
    Vt = sb("Vt", [P, 2, P], BF16)
    sm = sb("sm", [P, 8, 256], F32)
    pn = sb("pn", [P, 8, 256], BF16)
    PTs = sb("PTs", [P, 8, 2, P], BF16)
    sst = sb("sst", [P, 6, 8], F32)
    OT = sb("OT", [P, 4, P], BF16)
    gua = sb("gua", [P, 512], F32)
    gva = sb("gva", [P, 512], F32)
    sq = sb("sq", [P, 512], F32)
    vn = sb("vn", [P, 512], BF16)
    gmix = sb("gmix", [P, 512], BF16)
    gmT = sb("gmT", [P, 4, P], BF16)
    gst = sb("gst", [P, 6, 8], F32)
    lg = sb("lg", [P, 3, NE], F32)
    rst = sb("rst", [P, 16], F32)
    GTs = sb("GTs", [P, P], F32)
    Gpad = sb("Gpad", [P, P], F32)
    sbuf_s = [sb("silu%d" % i, [P, 512], F32) for i in range(2)]
    sbuf_u = [sb("up%d" % i, [P, 512], F32) for i in range(2)]
    actT = [sb("actT%d" % i, [P, 4, 512], BF16) for i in range(2)]
    arena = [sb("arena%d" % i, [P, SLOT], BF16) for i in range(3)]
    pst = [es.enter_context(nc.psum_tensor("ps%d" % i, [P, 512], F32)) for i in range(8)]

    ps_ctr = [0]

    def psum():
        i = ps_ctr[0] % 8
        ps_ctr[0] += 1
        return pst[i], ("ps", i)

    def DVE(fn, r=(), w=()):
        return cur[0].op("dve", fn, r, w)

    def ACT(fn, r=(), w=()):
        return cur[0].op("act", fn, r, w)

    def POOL(fn, r=(), w=()):
        return cur[0].op("pool", fn, r, w)

    def PE(fn, r=(), w=()):
        return cur[0].op("pe", fn, r, w)

    def DMA(eng, out, in_, r=(), w=()):
        return cur[0].op(eng, lambda e: e.dma_start(out=out, in_=in_), r, w, dma=True)

    def DMAD(eng, fn, r=(), w=(), slow=False):
        def f(e, g):
            o_, i_ = fn(g)
            return e.dma_start(out=o_, in_=i_, allow_slow_non_contiguous=slow)
        return cur[0].op(eng, f, r, w, dma=True, dyn=True)

    def mm(out, lhsT, rhs, start, stop, r, w):
        return PE(lambda e: e.matmul(out, lhsT=lhsT, rhs=rhs, start=start, stop=stop), r, w)

    XBf = XB[:, :, :].rearrange("p a b -> p (a b)")

    DMA("sp", id_f[:, :], ident_d, w=["id_f"])
    DMA("sp", mask2[:, :], mask_d, w=["mask2"])
    DMA("sp", pvec[:, :], pvec_d, w=["pvec"])
    for i in range(6):
        DMA("sp", lnbc[:, i, :], lnrows_d[i:i + 1, :].partition_broadcast(P), w=["lnbc"])
    DMA("sp", sinkb[:, :], sinks_d[0:1, :].partition_broadcast(P), w=["sinkb"])
    for i in range(2):
        DMA("sp", gnbc[:, i, :], gn_d[i:i + 1, :].partition_broadcast(P), w=["gnbc"])
    DMA("sp", bst[:, :], bst_d, w=["bst"])
    DMA("sp", wr[:, :, :], wr_d.rearrange("(k p) n -> p k n", p=P), w=["wr"])
    DMA("sp", brt[:, :], br_d[0:1, :].partition_broadcast(P), w=["brt"])
    DVE(lambda e: e.memset(bdn[:, :], 0.0), w=["bdn"])
    DMA("sp", bdn[0:NE, :], bdn_d, w=["bdn"])
    DMA("sp", bgu[:, :, :].rearrange("p a b -> p (a b)"), bgu_d, w=["bgu"])
    DMA("sp", bfm[:, :], bfm_d, w=["bfm"])
    DMA("sp", XB[:, 0, :], wst_d, w=[("XB", 0)])
    DMA("sp", xs[:, 0:P], tril_d, w=["xs"])
    DMA("sp", XB[0:1, 1:4, :].rearrange("p a b -> p (a b)")[:, 0:NTM + D], brow_d, w=[("XB", 1), ("XB", 2), ("XB", 3)])
    DVE(lambda e: e.tensor_copy(out=id_b[:, :], in_=id_f[:, :]), r=["id_f"], w=["id_b"])
    DVE(lambda e: e.memset(epst[:, :], EPS), w=["epst"])
    DVE(lambda e: e.memset(Gpad[:, :], 0.0), w=["Gpad"])
    DVE(lambda e: e.memset(ones_b[:, :], 1.0), w=["ones_b"])
    DVE(lambda e: e.memset(ones_f[:, :], 1.0), w=["ones_f"])
    DVE(lambda e: e.tensor_scalar(out=negsink[:, :], in0=sinkb[:, :], scalar1=-1.0, scalar2=None, op0=ALU.mult),
        r=["sinkb"], w=["negsink"])
    DVE(lambda e: e.tensor_tensor(out=wsb[:, :, :], in0=XB[:, 0, :].rearrange("p (h t) -> p h t", h=8),
                                  in1=xs[:, 0:P].unsqueeze(1).to_broadcast([P, 8, P]), op=ALU.mult),
        r=[("XB", 0), "xs"], w=["wsb"])
    DVE(lambda e: e.tensor_copy(out=brow[:, :], in_=XB[0:1, 1:4, :].rearrange("p a b -> p (a b)")[:, 0:NTM + D]),
        r=[("XB", 1), ("XB", 2), ("XB", 3)], w=["brow"])
    DVE(lambda e: e.tensor_scalar(out=bgu[:, :, 0:8], in0=bgu[:, :, 0:8], scalar1=GLU_A, scalar2=None, op0=ALU.mult),
        r=["bgu"], w=["bgu"])
    DVE(lambda e: e.tensor_scalar(out=bgu[:, :, 8:16], in0=bgu[:, :, 8:16], scalar1=1.0, scalar2=None, op0=ALU.add),
        r=["bgu"], w=["bgu"])

    DVE(lambda e: e.memset(kT[:, :, :, :], 0.0), w=["kT0"])
    DVE(lambda e: e.memset(Vt[:, :, :], 0.0), w=["Vt0"])
    cv_ctr = [0]

    cv_in = [arena[j // 3][:, :].bitcast(F32)[:, (j % 3) * 2048:(j % 3) * 2048 + 2048] for j in range(6)]
    cv_out = [arena[2][:, j * 2048:(j + 1) * 2048] for j in range(4)] + \
             [hT[:, 4 * j:4 * j + 4, :].rearrange("p a b -> p (a b)") for j in range(2)]

    def convert(src, dst, total):
        W = 2048 if total % (P * 2048) == 0 else 1024
        sf = src.rearrange("a b -> (a b)")
        df = dst.rearrange("a b -> (a b)")
        for i in range(total // (P * W)):
            k = cv_ctr[0]
            cv_ctr[0] += 1
            stg = cv_in[k % 6][:, 0:W]
            stb = cv_out[k % 6][:, 0:W]
            ik = [("cvi", k % 6)]
            ok = [("cvo", k % 6)]
            DMA("sp", stg, sf[i * P * W:(i + 1) * P * W].rearrange("(p n) -> p n", p=P), w=ik)
            which = k % 5
            if which in (0, 2):
                DVE(lambda e, stg=stg, stb=stb: e.tensor_copy(out=stb, in_=stg), r=ik, w=ok)
            elif which in (1, 3):
                ACT(lambda e, stg=stg, stb=stb: e.activation(out=stb, in_=stg, func=AF.Identity), r=ik, w=ok)
            else:
                POOL(lambda e, stg=stg, stb=stb: e.tensor_copy(out=stb, in_=stg), r=ik, w=ok)
            DMA("act", df[i * P * W:(i + 1) * P * W].rearrange("(p n) -> p n", p=P), stb, r=ok)

    def convert_all():
        convert(wfm_d, s_wfm, D * NFM)
        convert(wtm_d, s_wtm, D * NTM)
        convert(wout_d, s_wout, D * D)
        convert(wgu_d, s_gu, ne_decl * 2 * D * D)
        convert(wd_d, s_wd, ne_decl * D * D)

    items = [("fm", 0), ("tm", 0), ("wo", 0)] + [("ex", 0, i) for i in range(NITEM)]
    arena_pos = [0]
    item_slot = {}

    def akeys(s_):
        return [("agu", s_), ("agu2", s_), ("ad", s_)]

    def arena_load():
        k = arena_pos[0]
        arena_pos[0] += 1
        it = items[k]
        s_ = k % 3
        item_slot[it] = s_
        a = arena[s_]
        src, n = {"fm": (s_wfm, NFM), "tm": (s_wtm, NTM), "wo": (s_wout, D)}[it[0]]
        DMA("sp" if it[0] == "fm" else "act", a[:, 0:KC * n].rearrange("p (k n) -> p k n", k=KC), src.rearrange("(k p) n -> p k n", p=P), w=akeys(s_))

    def load_ex_gu(i):
        s_ = i % 3
        item_slot[("ex", 0, i)] = s_
        e_, hf = i // 2, i % 2
        r0 = (e_ * 2 + hf) * D
        gview = arena[s_][:, 0:KC * D].rearrange("p (k n) -> p k n", k=KC)
        sview = s_gu[r0:r0 + D, :].rearrange("(k p) n -> p k n", p=P)
        DMA("sp", gview[:, 0:6, :], sview[:, 0:6, :], w=[("agu", s_)])
        DMA("act", gview[:, 6:8, :], sview[:, 6:8, :], w=[("agu2", s_)])

    def load_ex_d(i):
        s_ = i % 3
        e_, hf = i // 2, i % 2
        r1 = e_ * D + hf * 512
        DMA("act", arena[s_][:, KC * D:KC * D + 4 * D].rearrange("p (m n) -> p m n", m=4),
            s_wd[r1:r1 + 512, :].rearrange("(m p) n -> p m n", p=P), w=[("ad", s_)])

    def layer_norm(src, dst, gi, rkeys, wkeys):
        for h in range(2):
            DVE(lambda e, h=h: e.bn_stats(out=lnst[:, 6 * h:6 * h + 6], in_=src[:, 512 * h:512 * h + 512]),
                r=rkeys, w=["lnst"])
        DVE(lambda e: e.bn_aggr(out=lnst[:, 12:14], in_=lnst[:, 0:12]), r=["lnst"], w=["lnmv"])
        ACT(lambda e: e.activation(out=lnst[:, 14:15], in_=lnst[:, 13:14], func=AF.Sqrt, bias=epst[:, 0:1], scale=1.0),
            r=["lnmv", "epst"], w=["lnsd"])
        DVE(lambda e: e.reciprocal(out=lnst[:, 15:16], in_=lnst[:, 14:15]), r=["lnsd"], w=["lnrs"])
        DVE(lambda e: e.tensor_scalar(out=dst, in0=src, scalar1=lnst[:, 12:13], scalar2=lnst[:, 15:16],
                                      op0=ALU.subtract, op1=ALU.mult), r=list(rkeys) + ["lnmv", "lnrs"], w=wkeys)
        DVE(lambda e: e.tensor_tensor(out=dst, in0=dst, in1=lnbc[:, gi, :], op=ALU.mult), r=list(wkeys) + ["lnbc"], w=wkeys)
        DVE(lambda e: e.tensor_tensor(out=dst, in0=dst, in1=lnbc[:, gi + 1, :], op=ALU.add), r=list(wkeys) + ["lnbc"], w=wkeys)

    def transpose_to_hT(b, also_f32):
        cols = slice(b * P, (b + 1) * P)
        for half in range(2):
            bank, bk = psum()
            for j in range(4):
                kc = half * 4 + j
                PE(lambda e, kc=kc, j=j, bank=bank: e.transpose(out=bank[:, j * P:(j + 1) * P],
                                                                 in_=XB[:, b, kc * P:(kc + 1) * P], identity=id_f[:, :]),
                   r=[("XB", b), "id_f"], w=[bk])
            ACT(lambda e, half=half, bank=bank: e.activation(
                out=hT[:, half * 4:half * 4 + 4, cols], in_=bank[:, :].rearrange("p (j t) -> p j t", j=4), func=AF.Identity),
                r=[bk], w=[("hT", b), ("evac", half)])
            if also_f32:
                DVE(lambda e, half=half, bank=bank: e.tensor_copy(
                    out=h1Tf[:, half * 4:half * 4 + 4, :], in_=bank[:, :].rearrange("p (j t) -> p j t", j=4)),
                    r=[bk, ("evac", half)], w=["h1Tf"])

    def load_x(b):
        DMAD("sp", lambda g, b=b: (xs[:, :], x_d[bass.ds(g * TG + b * P, P), :]), w=["xs"])

    def load_pos(b):
        DMAD("sp", lambda g, b=b: (posi[:, :], pos_d[0:1, bass.ds(g * TG + b * P, P)].partition_broadcast(P)), w=["posi"])

    def load_fmask():
        DMAD("sp", lambda g: (fmk[:, :], fmk_d[:, bass.ds(g, 1)]), w=["fmk"], slow=True)

    nblocks = NB
    stop_at = 9 if dbg is None else dbg.get("stop", 9)

    def mixer_block(g, b):
        gb = b
        first = False
        par = gb % 2
        cols = slice(b * P, (b + 1) * P)
        wfm = arena[item_slot[("fm", g)]][:, 0:KC * NFM].rearrange("p (k n) -> p k n", k=KC)
        wtm = arena[item_slot[("tm", g)]][:, 0:KC * NTM].rearrange("p (k n) -> p k n", k=KC)
        wo = arena[item_slot[("wo", g)]][:, 0:KC * D].rearrange("p (k n) -> p k n", k=KC)
        kfm = akeys(item_slot[("fm", g)])
        ktm = akeys(item_slot[("tm", g)])
        kwo = akeys(item_slot[("wo", g)])
        xbk = ("XB", b)
        hk = ("hT", b)

        layer_norm(xs[:, :], XB[:, b, :], 0, ["xs"], [xbk])
        if gb + 1 < nblocks:
            load_x(b + 1)
        transpose_to_hT(b, False)

        A_, U_, KF_, R_, C_, S_, U2_, KF2_ = [rp[:, i, :] for i in range(8)]
        DVE(lambda e: e.tensor_copy(out=A_, in_=posi[:, :]), r=["posi"], w=["rpA"])
        if gb + 1 < nblocks:
            load_pos(b + 1)
        DVE(lambda e: e.tensor_scalar(out=A_, in0=A_, scalar1=pvec[:, 0:1], scalar2=None, op0=ALU.mult), r=["rpA", "pvec"], w=["rpA"])
        DVE(lambda e: e.tensor_scalar(out=U_, in0=A_, scalar1=1.0 / TWO_PI, scalar2=None, op0=ALU.mult), r=["rpA"], w=["rpU"])
        DVE(lambda e: e.tensor_scalar(out=U2_, in0=U_, scalar1=0.25, scalar2=None, op0=ALU.add), r=["rpU"], w=["rpU2"])
        for (u_, kf_, ku, kk, shift, dst, kd) in ((U_, KF_, "rpU", "rpK", 0.0, S_, "rpS"), (U2_, KF2_, "rpU2", "rpK2", float(np.pi / 2), C_, "rpC")):
            DVE(lambda e, u_=u_: e.tensor_copy(out=rpi[:, :], in_=u_), r=[ku], w=["rpi"])
            DVE(lambda e, kf_=kf_: e.tensor_copy(out=kf_, in_=rpi[:, :]), r=["rpi"], w=[kk])
            DVE(lambda e, kf_=kf_: e.scalar_tensor_tensor(out=R_, in0=kf_, scalar=-CW1, in1=A_, op0=ALU.mult, op1=ALU.add),
                r=[kk, "rpA"], w=["rpR"])
            DVE(lambda e, kf_=kf_: e.scalar_tensor_tensor(out=R_, in0=kf_, scalar=-CW2, in1=R_, op0=ALU.mult, op1=ALU.add),
                r=[kk, "rpR"], w=["rpR"])
            if shift:
                DVE(lambda e, shift=shift: e.tensor_scalar(out=R_, in0=R_, scalar1=shift, scalar2=None, op0=ALU.add), r=["rpR"], w=["rpR"])
            DVE(lambda e: e.tensor_scalar(out=R_, in0=R_, scalar1=-PI_LO, scalar2=PI_LO, op0=ALU.max, op1=ALU.min), r=["rpR"], w=["rpR"])
            ACT(lambda e, dst=dst: e.activation(out=dst, in_=R_, func=AF.Sin), r=["rpR"], w=[kd])

        fb = []
        for j3 in range(3):
            bank, bk = psum()
            fb.append((bank, bk))
            for j in range(4):
                jj = j3 * 4 + j
                for kc in range(KC):
                    mm(bank[:, j * P:(j + 1) * P], wfm[:, kc, jj * P:(jj + 1) * P], hT[:, kc, cols], kc == 0, kc == KC - 1,
                       kfm + [hk], [bk])
        tb_ = [psum() for _ in range(3)]
        tsl = [(0, 128), (128, 640), (640, 1152)]
        for kc in range(KC):
            for (bank, bk), (c0, c1) in zip(tb_, tsl):
                mm(bank[:, 0:c1 - c0], hT[:, kc, cols], wtm[:, kc, c0:c1], kc == 0, False, ktm + [hk], [bk])
        for (bank, bk), (c0, c1) in zip(tb_, tsl):
            mm(bank[:, 0:c1 - c0], ones_b[0:1, :], brow[0:1, c0:c1], False, True, ["ones_b", "brow"], [bk])

        if stop_at <= 1:
            return
        for j in range(6):
            if j < 4:
                (qb, qk), qo = fb[0], j * P
                (sbk_, sk), so = fb[1], j * P
                bq, bs_ = j, 4 + j
                dst, dk = qT[:, j, :], "qT"
            else:
                (qb, qk), qo = fb[2], (j - 4) * P
                (sbk_, sk), so = fb[2], (j - 2) * P
                bq, bs_ = 8 + (j - 4), 10 + (j - 4)
                dst, dk = kT[:, par, j - 4, :], ("kT", par)
            DVE(lambda e, qb=qb, qo=qo, bq=bq, j=j: e.scalar_tensor_tensor(
                out=t1s[:, j % 2, :], in0=qb[:, qo:qo + P], scalar=bfm[:, bq:bq + 1], in1=C_, op0=ALU.add, op1=ALU.mult),
                r=[qk, "bfm", "rpC"], w=[("t1", j % 2)])
            DVE(lambda e, sbk_=sbk_, so=so, bs_=bs_, j=j: e.scalar_tensor_tensor(
                out=t2s[:, j % 2, :], in0=sbk_[:, so:so + P], scalar=bfm[:, bs_:bs_ + 1], in1=S_, op0=ALU.add, op1=ALU.mult),
                r=[sk, "bfm", "rpS"], w=[("t2", j % 2)])
            DVE(lambda e, dst=dst, j=j: e.scalar_tensor_tensor(
                out=dst, in0=t2s[:, j % 2, :], scalar=pvec[:, 1:2], in1=t1s[:, j % 2, :], op0=ALU.mult, op1=ALU.add),
                r=[("t1", j % 2), ("t2", j % 2), "pvec"], w=[dk])
        ACT(lambda e: e.activation(out=Vt[:, par, :], in_=tb_[0][0][:, 0:P], func=AF.Identity), r=[tb_[0][1]], w=[("Vt", par)])
        ACT(lambda e: e.activation(out=gua[:, :], in_=tb_[1][0][:, :], func=AF.Gelu), r=[tb_[1][1]], w=["gua"])
        ACT(lambda e: e.activation(out=gva[:, :], in_=tb_[2][0][:, :], func=AF.Gelu), r=[tb_[2][1]], w=["gva"])

        if stop_at <= 2:
            return
        koff = P if first else 0
        nk = 256 - koff
        scb = [psum() for _ in range(4)]
        SLOT_HEAD = (0, 2, 4, 6, 1, 3, 5, 7)
        for sl in range(8):
            hq = SLOT_HEAD[sl]
            c, half, kvg = hq // 2, hq % 2, hq // 4
            bank, bk = scb[sl // 2]
            off = (sl % 2) * 256
            prt = slice(half * 64, half * 64 + 64)
            if not first:
                mm(bank[:, off:off + P], qT[prt, c, :], kT[prt, 1 - par, kvg, :], True, True, ["qT", ("kT", 1 - par)], [bk])
            mm(bank[:, off + P:off + 256], qT[prt, c, :], kT[prt, par, kvg, :], True, True, ["qT", ("kT", par)], [bk])
        for j in range(4):
            bank, bk = scb[j]
            DVE(lambda e, bank=bank, j=j: e.scalar_tensor_tensor(
                out=sm[:, 2 * j:2 * j + 2, koff:256], in0=bank[:, :].rearrange("p (h k) -> p h k", h=2)[:, :, koff:256],
                scalar=0.125, in1=mask2[:, koff:256].unsqueeze(1).to_broadcast([P, 2, nk]), op0=ALU.mult, op1=ALU.add),
                r=[bk, "mask2"], w=[("sm", j)])
        smk = [("sm", j) for j in range(4)]
        if b == 0:
            DVE(lambda e: e.tensor_scalar(out=sm[:, :, 0:P], in0=sm[:, :, 0:P], scalar1=fmk[:, 0:1], scalar2=None, op0=ALU.add),
                r=smk + ["fmk"], w=smk)
        for j in range(4):
            DVE(lambda e, j=j: e.tensor_reduce(out=sst[:, 0, 2 * j:2 * j + 2], in_=sm[:, 2 * j:2 * j + 2, koff:256], axis=AX.X, op=ALU.max),
                r=[("sm", j)], w=[("mx", j)])
        DVE(lambda e: e.scalar_tensor_tensor(out=sst[:, 1, :], in0=sst[:, 0, :], scalar=-1.0, in1=negsink[:, :], op0=ALU.mult, op1=ALU.min),
            r=[("mx", j) for j in range(4)] + ["negsink"], w=["negm"])
        for sl in range(8):
            ACT(lambda e, sl=sl: e.activation(out=sm[:, sl, koff:256], in_=sm[:, sl, koff:256], func=AF.Exp,
                                              bias=sst[:, 1, sl:sl + 1], scale=1.0),
                r=[("sm", sl // 2), "negm"], w=[("sm", sl // 2)])
        DVE(lambda e: e.tensor_reduce(out=sst[:, 2, :], in_=sm[:, :, koff:256], axis=AX.X, op=ALU.add), r=smk, w=["rs"])
        DVE(lambda e: e.tensor_tensor(out=sst[:, 3, :], in0=sst[:, 1, :], in1=sinkb[:, :], op=ALU.add), r=["negm", "sinkb"], w=["es"])
        ACT(lambda e: e.activation(out=sst[:, 3, :], in_=sst[:, 3, :], func=AF.Exp), r=["es"], w=["es"])
        DVE(lambda e: e.tensor_tensor(out=sst[:, 4, :], in0=sst[:, 2, :], in1=sst[:, 3, :], op=ALU.add), r=["rs", "es"], w=["den"])
        DVE(lambda e: e.reciprocal(out=sst[:, 5, :], in_=sst[:, 4, :]), r=["den"], w=["rcp"])
        DVE(lambda e: e.tensor_tensor(out=pn[:, :, koff:256], in0=sm[:, :, koff:256],
                                      in1=sst[:, 5, :].unsqueeze(2).to_broadcast([P, 8, nk]), op=ALU.mult),
            r=smk + ["rcp"], w=["pn"])
        ptb = [psum() for _ in range(2)]
        for hq in range(8):
            bank, bk = ptb[hq // 4]
            bv = bank[:, :].bitcast(BF16)
            for kb in range(2):
                if first and kb == 0:
                    continue
                o0 = ((hq % 4) * 2 + kb) * P
                PE(lambda e, bv=bv, o0=o0, hq=hq, kb=kb: e.transpose(out=bv[:, o0:o0 + P], in_=pn[:, hq, kb * P:(kb + 1) * P], identity=id_b[:, :]),
                   r=["pn", "id_b"], w=[bk])
        k0 = 1 if first else 0
        for j in range(2):
            bank, bk = ptb[j]
            bv = bank[:, :].bitcast(BF16).rearrange("p (h k t) -> p h k t", h=4, k=2)
            ACT(lambda e, bv=bv, j=j: e.activation(out=PTs[:, 4 * j:4 * j + 4, k0:2, :], in_=bv[:, :, k0:2, :], func=AF.Identity),
                r=[bk], w=[("PTs", j)])
        ob, obk = psum()
        for sl in range(8):
            hq = SLOT_HEAD[sl]
            c, half, kvg = hq // 2, hq % 2, hq // 4
            o_ap = ob[half * 64:half * 64 + 64, c * P:(c + 1) * P]
            if not first:
                mm(o_ap, Vt[:, 1 - par, kvg * 64:kvg * 64 + 64], PTs[:, sl, 0, :], True, False, [("Vt", 1 - par), ("PTs", sl // 4)], [obk])
            mm(o_ap, Vt[:, par, kvg * 64:kvg * 64 + 64], PTs[:, sl, 1, :], first, True, [("Vt", par), ("PTs", sl // 4)], [obk])
        ACT(lambda e: e.activation(out=OT[:, :, :], in_=ob[:, :].rearrange("p (c t) -> p c t", c=4), func=AF.Identity), r=[obk], w=["OT"])

        if stop_at <= 3:
            return
        gva3 = gva[:, :].rearrange("p (h d) -> p h d", h=8)
        sq3 = sq[:, :].rearrange("p (h d) -> p h d", h=8)
        DVE(lambda e: e.tensor_reduce(out=gst[:, 0, :], in_=gva3, axis=AX.X, op=ALU.add), r=["gva"], w=["gs1"])
        ACT(lambda e: e.activation(out=sq[:, :], in_=gva[:, :], func=AF.Square), r=["gva"], w=["sq"])
        DVE(lambda e: e.tensor_reduce(out=gst[:, 1, :], in_=sq3, axis=AX.X, op=ALU.add), r=["sq"], w=["gs2"])
        DVE(lambda e: e.tensor_scalar(out=gst[:, 2, :], in0=gst[:, 0, :], scalar1=1.0 / 64, scalar2=None, op0=ALU.mult), r=["gs1"], w=["gmean"])
        DVE(lambda e: e.tensor_tensor(out=gst[:, 3, :], in0=gst[:, 2, :], in1=gst[:, 2, :], op=ALU.mult), r=["gmean"], w=["gmsq"])
        DVE(lambda e: e.scalar_tensor_tensor(out=gst[:, 4, :], in0=gst[:, 1, :], scalar=1.0 / 64, in1=gst[:, 3, :], op0=ALU.mult, op1=ALU.subtract),
            r=["gs2", "gmsq"], w=["gvar"])
        ACT(lambda e: e.activation(out=gst[:, 4, :], in_=gst[:, 4, :], func=AF.Sqrt, bias=epst[:, 0:1], scale=1.0), r=["gvar", "epst"], w=["gvar"])
        DVE(lambda e: e.reciprocal(out=gst[:, 5, :], in_=gst[:, 4, :]), r=["gvar"], w=["grstd"])
        DVE(lambda e: e.tensor_tensor(out=gva3, in0=gva3, in1=gst[:, 2, :].unsqueeze(2).to_broadcast([P, 8, 64]), op=ALU.subtract),
            r=["gva", "gmean"], w=["gva"])
        DVE(lambda e: e.tensor_tensor(out=gva3, in0=gva3, in1=gst[:, 5, :].unsqueeze(2).to_broadcast([P, 8, 64]), op=ALU.mult),
            r=["gva", "grstd"], w=["gva"])
        DVE(lambda e: e.tensor_tensor(out=gva3, in0=gva3, in1=gnbc[:, 0, :].unsqueeze(1).to_broadcast([P, 8, 64]), op=ALU.mult),
            r=["gva", "gnbc"], w=["gva"])
        DVE(lambda e: e.tensor_tensor(out=vn[:, :].rearrange("p (h d) -> p h d", h=8), in0=gva3,
                                      in1=gnbc[:, 1, :].unsqueeze(1).to_broadcast([P, 8, 64]), op=ALU.add),
            r=["gva", "gnbc"], w=["vn"])
        spb, spk = psum()
        for h in range(8):
            mm(spb[:, h * 64:h * 64 + 64], wsb[:, h, :], vn[:, h * 64:h * 64 + 64], True, True, ["wsb", "vn"], [spk])
        DVE(lambda e: e.tensor_tensor(out=sq3, in0=spb[:, :].rearrange("p (h d) -> p h d", h=8),
                                      in1=bst[:, :].unsqueeze(2).to_broadcast([P, 8, 64]), op=ALU.add), r=[spk, "bst"], w=["sq"])
        DVE(lambda e: e.tensor_tensor(out=gmix[:, :], in0=sq[:, :], in1=gua[:, :], op=ALU.mult), r=["sq", "gua"], w=["gmix"])
        gtb, gtk = psum()
        gtv = gtb[:, :].bitcast(BF16)
        for c in range(4):
            PE(lambda e, c=c: e.transpose(out=gtv[:, c * P:(c + 1) * P], in_=gmix[:, c * P:(c + 1) * P], identity=id_b[:, :]),
               r=["gmix", "id_b"], w=[gtk])
        ACT(lambda e: e.activation(out=gmT[:, :, :], in_=gtv[:, 0:4 * P].rearrange("p (c t) -> p c t", c=4), func=AF.Identity), r=[gtk], w=["gmT"])

        if stop_at <= 4:
            return
        for half in range(2):
            bank, bk = psum()
            hs = slice(half * 512, half * 512 + 512)
            for c in range(4):
                mm(bank[:, :], OT[:, c, :], wo[:, c, hs], c == 0, False, ["OT"] + kwo, [bk])
            for c in range(4):
                mm(bank[:, :], gmT[:, c, :], wo[:, 4 + c, hs], False, False, ["gmT"] + kwo, [bk])
            mm(bank[:, :], ones_b[0:1, :], brow[0:1, NTM + half * 512:NTM + half * 512 + 512], False, True, ["ones_b", "brow"], [bk])
            DVE(lambda e, bank=bank, hs=hs: e.scalar_tensor_tensor(out=XB[:, b, hs], in0=XB[:, b, hs], scalar=ALPHA, in1=bank[:, :],
                                                                 op0=ALU.mult, op1=ALU.add), r=[bk, xbk], w=[xbk])
        layer_norm(XB[:, b, :], XB[:, b, :], 2, [xbk], [xbk])
        transpose_to_hT(b, True)

        if stop_at <= 5:
            return
        rb, rk = psum()
        for kc in range(KC):
            mm(rb[:, 0:NE], h1Tf[:, kc, :], wr[:, kc, :], kc == 0, kc == KC - 1, ["h1Tf", "wr"], [rk])
        DVE(lambda e: e.tensor_tensor(out=lg[:, 0, :], in0=rb[:, 0:NE], in1=brt[:, :], op=ALU.add), r=[rk, "brt"], w=["lg0"])
        DVE(lambda e: e.max(out=rst[:, 0:8], in_=lg[:, 0, :]), r=["lg0"], w=["mx8"])
        DVE(lambda e: e.tensor_scalar(out=lg[:, 1, :], in0=lg[:, 0, :], scalar1=rst[:, 3:4], scalar2=None, op0=ALU.is_ge), r=["lg0", "mx8"], w=["lg1"])
        DVE(lambda e: e.tensor_scalar(out=rst[:, 8:9], in0=rst[:, 0:1], scalar1=-1.0, scalar2=None, op0=ALU.mult), r=["mx8"], w=["nm1"])
        ACT(lambda e: e.activation(out=lg[:, 2, :], in_=lg[:, 0, :], func=AF.Exp, bias=rst[:, 8:9], scale=1.0), r=["lg0", "nm1"], w=["lg2"])
        DVE(lambda e: e.tensor_tensor(out=lg[:, 2, :], in0=lg[:, 2, :], in1=lg[:, 1, :], op=ALU.mult), r=["lg2", "lg1"], w=["lg2"])
        DVE(lambda e: e.tensor_reduce(out=rst[:, 9:10], in_=lg[:, 2, :], axis=AX.X, op=ALU.add), r=["lg2"], w=["ssum"])
        DVE(lambda e: e.reciprocal(out=rst[:, 10:11], in_=rst[:, 9:10]), r=["ssum"], w=["rc"])
        DVE(lambda e: e.tensor_scalar(out=Gpad[:, 0:NE], in0=lg[:, 2, :], scalar1=rst[:, 10:11], scalar2=None, op0=ALU.mult), r=["lg2", "rc"], w=["Gpad"])
        DVE(lambda e: e.tensor_copy(out=G[:, b, :], in_=Gpad[:, 0:NE]), r=["Gpad"], w=[("G", b)])
        DVE(lambda e: e.tensor_scalar(out=Gs[:, b, :], in0=Gpad[:, 0:NE], scalar1=1.0 / GLU_A, scalar2=None, op0=ALU.mult), r=["Gpad"], w=[("Gs", b)])
        gb_, gk_ = psum()
        PE(lambda e: e.transpose(out=gb_[:, 0:P], in_=Gpad[:, :], identity=id_f[:, :]), r=["Gpad", "id_f"], w=[gk_])
        ACT(lambda e: e.activation(out=GTs[:, :], in_=gb_[:, 0:P], func=AF.Identity), r=[gk_], w=["GTs"])
        for half in range(2):
            bank, bk = psum()
            hs = slice(half * 512, half * 512 + 512)
            mm(bank[:, :], GTs[:, :], bdn[:, hs], True, True, ["GTs", "bdn"], [bk])
            DVE(lambda e, bank=bank, hs=hs: e.scalar_tensor_tensor(out=XB[:, b, hs], in0=XB[:, b, hs], scalar=ALPHA, in1=bank[:, :],
                                                                 op0=ALU.mult, op1=ALU.add), r=[bk, xbk], w=[xbk])

    hkeys = [("hT", b) for b in range(NB)]
    nch = TG // 512

    def moe_gu(g, i):
        e_, hf = i // 2, i % 2
        s = item_slot[("ex", g, i)]
        ak = ("agu", s)
        wgu = arena[s][:, 0:KC * D].rearrange("p (k n) -> p k n", k=KC)
        for ch in range(nch):
            tk = slice(ch * 512, ch * 512 + 512)
            ab = actT[(i * nch + ch) % 2]
            abk = ("actT", (i * nch + ch) % 2)
            for m in range(4):
                pg, pgk = psum()
                pu, puk = psum()
                for kc in range(KC):
                    mm(pg[:, :], wgu[:, kc, m * P:(m + 1) * P], hT[:, kc, tk], kc == 0, kc == KC - 1, [ak, ("agu2", s)] + hkeys[ch * 4:ch * 4 + 4], [pgk])
                for kc in range(KC):
                    mm(pu[:, :], wgu[:, kc, 512 + m * P:512 + (m + 1) * P], hT[:, kc, tk], kc == 0, kc == KC - 1, [ak, ("agu2", s)] + hkeys[ch * 4:ch * 4 + 4], [puk])
                j = hf * 4 + m
                sbi = (m % 2)
                sS, sU = sbuf_s[sbi], sbuf_u[sbi]
                ACT(lambda e, pg=pg, sS=sS, j=j: e.activation(out=sS[:, :], in_=pg[:, :], func=AF.Silu, bias=bgu[:, e_, j:j + 1], scale=GLU_A),
                    r=[pgk, "bgu"], w=[("sS", sbi)])
                ACT(lambda e, pu=pu, sU=sU, j=j: e.activation(out=sU[:, :], in_=pu[:, :], func=AF.Identity, bias=bgu[:, e_, 8 + j:9 + j], scale=1.0),
                    r=[puk, "bgu"], w=[("sU", sbi)])
                DVE(lambda e, sU=sU: e.tensor_scalar(out=sU[:, :], in0=sU[:, :], scalar1=-6.0, scalar2=8.0, op0=ALU.max, op1=ALU.min),
                    r=[("sU", sbi)], w=[("sU", sbi)])
                DVE(lambda e, sS=sS, sU=sU, ab=ab, m=m: e.scalar_tensor_tensor(out=ab[:, m, :], in0=sS[:, :], scalar=C7, in1=sU[:, :],
                                                                              op0=ALU.min, op1=ALU.mult),
                    r=[("sS", sbi), ("sU", sbi)], w=[abk])

    def moe_down(g, i):
        e_, hf = i // 2, i % 2
        s = item_slot[("ex", g, i)]
        ak = ("ad", s)
        wdn = arena[s][:, KC * D:KC * D + 4 * D].rearrange("p (m n) -> p m n", m=4)
        for ch in range(nch):
            ab = actT[(i * nch + ch) % 2]
            abk = ("actT", (i * nch + ch) % 2)
            for tbk in range(4):
                b = ch * 4 + tbk
                for half in range(2):
                    hs = slice(half * 512, half * 512 + 512)
                    pd, pdk = psum()
                    for m in range(4):
                        mm(pd[:, :], ab[:, m, tbk * P:(tbk + 1) * P], wdn[:, m, hs], m == 0, m == 3, [abk, ak], [pdk])
                    DVE(lambda e, pd=pd, hs=hs, b=b: e.scalar_tensor_tensor(out=XB[:, b, hs], in0=pd[:, :], scalar=Gs[:, b, e_:e_ + 1],
                                                                          in1=XB[:, b, hs], op0=ALU.mult, op1=ALU.add),
                        r=[pdk, ("Gs", b), ("XB", b)], w=[("XB", b)])

    def ln2_out(g):
        for b in range(NB):
            o = ost[b % 2]
            ok = ("ost", b % 2)
            layer_norm(XB[:, b, :], o[:, :], 4, [("XB", b)], [ok])
            DMAD("sp", lambda g, b=b, o=o: (out_d[bass.ds(g * TG + b * P, P), :], o[:, :]), r=[ok])

    stage = 9 if dbg is None else dbg.get("stage", 9)
    convert_all()
    cur[0] = S1
    load_x(0)
    load_pos(0)
    load_fmask()
    arena_load(); arena_load(); arena_load()
    g = 0
    if stage >= 1:
        for b in range(NB if dbg is None else dbg.get("nb", NB)):
            mixer_block(g, b)
    if stage >= 2:
        nit = NITEM if dbg is None else dbg.get("nitem", NITEM)
        for i in range(min(3, nit)):
            load_ex_gu(i)
            load_ex_d(i)
        moe_gu(g, 0)
        if 3 < nit:
            load_ex_gu(3)
        for i in range(nit):
            if i + 1 < nit:
                moe_gu(g, i + 1)
                if i + 4 < nit:
                    load_ex_gu(i + 4)
            moe_down(g, i)
            if i + 3 < nit:
                load_ex_d(i + 3)
    if stage >= 3:
        ln2_out(g)
    if dbg and dbg.get("fn"):
        dbg["fn"](cur[0], DMA, dbg_d, locals())

    f0 = S0.finalize()
    f1 = S1.finalize()
    sems = {}
    for nm in sorted(set(f0) | set(f1)):
        sems[nm] = es.enter_context(nc.semaphore("s_%s_%s_%d" % nm))
    bar_a = es.enter_context(nc.semaphore("bar_a"))
    bar_b = es.enter_context(nc.semaphore("bar_b"))

    def emit_engine(name, h):
        S0.emit(name, h, sems)
        S0.barrier(name, h, sems, bar_a, bar_b, 5)
        with h.Fori(0, ng) as gi:
            S1.emit(name, h, sems, gi)
            S1.barrier(name, h, sems, bar_a, bar_b, gi * 5 + 10)

    with nc.Block() as block:
        @block.tensor
        def _(e):
            emit_engine("pe", e)

        @block.vector
        def _(e):
            emit_engine("dve", e)

        @block.scalar
        def _(e):
            emit_engine("act", e)

        @block.gpsimd
        def _(e):
            emit_engine("pool", e)

        @block.sync
        def _(e):
            emit_engine("sp", e)
    es.close()
    return nc


def _partner(ch):
    return ch + 8 if ch < 8 else (ch - 8 if ch < 16 else ch)


def _host_inputs(inp):
    f = np.float32
    w_in = np.asarray(inp["w_in"], f)[0]
    b_in = np.asarray(inp["b_in"], f)[0]
    q_cols = np.arange(512)
    qs_cols = np.array([(c // 64) * 64 + _partner(c % 64) for c in range(512)])
    k0 = 512 + np.arange(64)
    k1 = 576 + np.arange(64)
    k0s = 512 + np.array([_partner(c) for c in range(64)])
    k1s = 576 + np.array([_partner(c) for c in range(64)])
    fm_cols = np.concatenate([q_cols, qs_cols, k0, k0, k1, k1, k0s, k0s, k1s, k1s])
    tm_cols = np.arange(640, 1792)
    inv_freq = (f(500000.0) ** (-np.arange(0, 16, 2, dtype=f) / f(16))).astype(f)
    pvec = np.zeros((P, 2), f)
    for p in range(P):
        ch = p % 64
        if ch < 16:
            pvec[p, 0] = inv_freq[ch % 8]
            pvec[p, 1] = -1.0 if ch < 8 else 1.0
    qi = np.arange(P)[:, None]
    kj = np.arange(P)[None, :]
    mask2 = np.concatenate([np.where(kj > qi, 0.0, NEG), np.where(kj <= qi, 0.0, NEG)], axis=1).astype(f)
    tril = (kj >= qi).astype(f)
    wgu = np.asarray(inp["w_gate_up"], f)[0].reshape(NE, D, 2, 2, 512).transpose(0, 3, 1, 2, 4)
    shared = {
        "ident": np.eye(P, dtype=f),
        "mask2": mask2,
        "tril": tril,
        "pvec": pvec,
        "fmk": np.ascontiguousarray(np.broadcast_to(np.where(np.arange(NG) % (SEQ // TG) == 0, NEG, 0.0).astype(f)[None, :], (P, NG))),
        "lnrows": np.stack([np.asarray(inp["ln_in_g"], f), np.asarray(inp["ln_in_b"], f),
                            np.asarray(inp["ln1_g"], f)[0], np.asarray(inp["ln1_b"], f)[0],
                            np.asarray(inp["ln2_g"], f)[0], np.asarray(inp["ln2_b"], f)[0]]),
        "w_in_fm": np.ascontiguousarray(w_in[:, fm_cols]),
        "w_in_tm": np.ascontiguousarray(w_in[:, tm_cols]),
        "w_out": np.ascontiguousarray(np.asarray(inp["w_out"], f)[0]),
        "b_fm": np.ascontiguousarray(b_in[fm_cols].reshape(12, P).T),
        "b_rows": np.concatenate([b_in[tm_cols], np.asarray(inp["b_out"], f)[0]])[None, :],
        "sinks": np.ascontiguousarray(np.asarray(inp["sinks"], f).reshape(8)[[0, 2, 4, 6, 1, 3, 5, 7]]).reshape(1, 8),
        "gn": np.stack([np.asarray(inp["gn_g"], f)[0], np.asarray(inp["gn_b"], f)[0]]),
        "wsT": np.ascontiguousarray(np.asarray(inp["w_s"], f)[0].transpose(2, 0, 1)).reshape(P, 8 * P),
        "bsT": np.ascontiguousarray(np.asarray(inp["b_s"], f)[0].T),
        "w_router": np.ascontiguousarray(np.asarray(inp["w_router"], f)[0]),
        "b_router": np.asarray(inp["b_router"], f).reshape(1, NE),
        "w_gu": np.ascontiguousarray(wgu).reshape(NE * 2 * D, D),
        "w_down": np.ascontiguousarray(np.asarray(inp["w_down"], f)[0]).reshape(NE * D, D),
        "b_gu": np.ascontiguousarray(np.asarray(inp["b_gate_up"], f)[0].reshape(NE, 16, P).transpose(2, 0, 1)).reshape(P, NE * 16),
        "b_down": np.ascontiguousarray(np.asarray(inp["b_down"], f)[0]),
    }
    x = np.asarray(inp["x"], f).reshape(-1, D)
    pos = np.asarray(inp["positions"], np.int32).reshape(1, -1)
    maps = []
    for c in range(8):
        m = dict(shared)
        m["x"] = np.ascontiguousarray(x[c * TOK:(c + 1) * TOK])
        m["pos"] = np.ascontiguousarray(pos[:, c * TOK:(c + 1) * TOK])
        maps.append(m)
    return maps


def kernel(**inputs):
    ng = int(inputs.pop("_ng", NG))
    maps = _host_inputs(inputs)
    nc = build_program(ng=ng)
    res = run_bass_kernel_spmd(nc, maps, core_ids=list(range(8)))
    out = np.concatenate([np.asarray(r["out"], np.float32) for r in res.results], axis=0)
    return out.reshape(16, SEQ, D)
```
